# Optimizing a Trainium2 kernel written in Bass

```python
import math
import jax, jax.numpy as jnp
from jax import lax
import numpy as np

D_MODEL = 1024
BATCH = 8
SEQ = 4096
DEPTH = 4

N_MIXERS = 3
N_Q_HEADS = 16
N_KV_HEADS = 4
HEAD_DIM = D_MODEL // N_Q_HEADS
Q_BLOCK = 128
ROPE_THETA = 10000.0
GRID_W = 64
GMLP_CHUNK = 128
GMLP_WIDTH = 2 * D_MODEL
GMLP_GROUPS = 8
CONV_WIDTH = 3
N_EXPERTS = 32
TOP_K = 4
D_FF = D_MODEL
SWIGLU_ALPHA = 1.702
SWIGLU_LIMIT = 7.0
EXPERT_BLOCK = 128
LN_EPS = 1e-5
QK_EPS = 1e-6
DEEPNORM_ALPHA = (2 * DEPTH) ** 0.25
DEEPNORM_BETA = (8 * DEPTH) ** -0.25

kernel_name = "hybrid_interleaved_encoder_moe"


def _layers_of_kind(kind):
    return len(range(kind, DEPTH, N_MIXERS))


def layer_norm(x, g, b):
    xf = x.astype(jnp.float32)
    mu = jnp.mean(xf, -1, keepdims=True)
    var = jnp.mean(jnp.square(xf - mu), -1, keepdims=True)
    return ((xf - mu) * lax.rsqrt(var + LN_EPS) * g + b).astype(x.dtype)


def rms_norm(x, g):
    xf = x.astype(jnp.float32)
    return (xf * lax.rsqrt(jnp.mean(xf * xf, -1, keepdims=True) + QK_EPS) * g).astype(x.dtype)


def axial_rope_angles(seq_len):
    rows = seq_len // GRID_W
    grid = jnp.arange(rows * GRID_W, dtype=jnp.int32).reshape(rows, GRID_W)
    row = (grid // GRID_W).reshape(-1).astype(jnp.float32)
    col = (grid % GRID_W).reshape(-1).astype(jnp.float32)
    n_pairs = HEAD_DIM // 4
    inv = ROPE_THETA ** (-jnp.arange(n_pairs, dtype=jnp.float32) / n_pairs)
    ang = jnp.concatenate([row[:, None] * inv, col[:, None] * inv], -1)
    return jnp.cos(ang), jnp.sin(ang)


def apply_rope(x, cos, sin):
    xf = x.astype(jnp.float32).reshape(*x.shape[:-1], HEAD_DIM // 2, 2)
    x0, x1 = xf[..., 0], xf[..., 1]
    c, s = cos[:, None, :], sin[:, None, :]
    out = jnp.stack([x0 * c - x1 * s, x0 * s + x1 * c], -1)
    return out.reshape(x.shape).astype(x.dtype)


def attention_mixer(x, w_qkv, q_norm, k_norm, w_o):
    bsz, seq, _ = x.shape
    groups = N_Q_HEADS // N_KV_HEADS
    qkv = x @ w_qkv
    q, k, v = jnp.split(qkv, [N_Q_HEADS * HEAD_DIM, (N_Q_HEADS + N_KV_HEADS) * HEAD_DIM], axis=-1)
    q = rms_norm(q.reshape(bsz, seq, N_Q_HEADS, HEAD_DIM), q_norm)
    k = rms_norm(k.reshape(bsz, seq, N_KV_HEADS, HEAD_DIM), k_norm)
    v = v.reshape(bsz, seq, N_KV_HEADS, HEAD_DIM)
    cos, sin = axial_rope_angles(seq)
    q = apply_rope(q, cos, sin)
    k = apply_rope(k, cos, sin)
    n_blocks = seq // Q_BLOCK
    qb = q.reshape(bsz, n_blocks, Q_BLOCK, N_KV_HEADS, groups, HEAD_DIM).transpose(1, 0, 2, 3, 4, 5)
    scale = HEAD_DIM ** -0.5

    def query_block(q_blk):
        s = jnp.einsum('bqkgd,bskd->bkgqs', q_blk, k).astype(jnp.float32) * scale
        p = jax.nn.softmax(s, axis=-1).astype(v.dtype)
        return jnp.einsum('bkgqs,bskd->bqkgd', p, v)

    o = lax.map(query_block, qb)
    o = o.transpose(1, 0, 2, 3, 4, 5).reshape(bsz, seq, N_Q_HEADS * HEAD_DIM)
    return o @ w_o


def gmlp_mixer(x, w_in, norm_g, norm_b, w_s, b_s, w_out):
    bsz, seq, _ = x.shape
    z = jax.nn.gelu(x @ w_in, approximate=False)
    u, v = jnp.split(z, 2, axis=-1)
    v = layer_norm(v, norm_g, norm_b)
    n_chunks = seq // GMLP_CHUNK
    gw = GMLP_WIDTH // GMLP_GROUPS
    v = v.reshape(bsz, n_chunks, GMLP_CHUNK, GMLP_GROUPS, gw)
    mixed = jnp.einsum('gpq,bcqgd->bcpgd', w_s, v) + b_s.T[:, :, None]
    return (u * mixed.reshape(bsz, seq, GMLP_WIDTH)) @ w_out


def shortconv_mixer(x, w_in, conv_w, w_out):
    h = x @ w_in
    b_gate, c_gate, xv = jnp.split(h, 3, axis=-1)
    cx = c_gate * xv
    pad = jnp.pad(cx, ((0, 0), (1, 1), (0, 0)))
    y = pad[:, :-2] * conv_w[0] + pad[:, 1:-1] * conv_w[1] + pad[:, 2:] * conv_w[2]
    return (b_gate * y) @ w_out


def clamped_swiglu(h):
    h_glu = jnp.minimum(h[..., 0::2], SWIGLU_LIMIT)
    h_lin = jnp.clip(h[..., 1::2], -SWIGLU_LIMIT, SWIGLU_LIMIT)
    return h_glu * jax.nn.sigmoid(SWIGLU_ALPHA * h_glu) * (h_lin + 1.0)


def moe_ffn(x, router_w, router_b, w_gate_up, b_gate_up, w_down, b_down):
    bsz, seq, d = x.shape
    n_tok = bsz * seq
    xt = x.reshape(n_tok, d)
    logits = (xt @ router_w + router_b).astype(jnp.float32)
    top_logits, top_idx = lax.top_k(logits, TOP_K)
    gates = jax.nn.softmax(top_logits, axis=-1)
    n_assign = n_tok * TOP_K
    flat_e = top_idx.reshape(-1)
    order = jnp.argsort(flat_e)
    sorted_e = flat_e[order]
    sorted_tok = (order // TOP_K).astype(jnp.int32)
    sorted_gate = gates.reshape(-1)[order]
    counts = jnp.bincount(flat_e, length=N_EXPERTS)
    padded = (counts + EXPERT_BLOCK - 1) // EXPERT_BLOCK * EXPERT_BLOCK
    pad_end = jnp.cumsum(padded)
    pad_start = pad_end - padded
    start = jnp.cumsum(counts) - counts
    dest = pad_start[sorted_e] + jnp.arange(n_assign, dtype=jnp.int32) - start[sorted_e]
    n_blocks = -(-(n_assign + N_EXPERTS * (EXPERT_BLOCK - 1)) // EXPERT_BLOCK)
    n_rows = n_blocks * EXPERT_BLOCK
    row_tok = jnp.full((n_rows,), n_tok, jnp.int32).at[dest].set(sorted_tok)
    row_gate = jnp.zeros((n_rows,), jnp.float32).at[dest].set(sorted_gate)
    block_start = jnp.arange(n_blocks, dtype=jnp.int32) * EXPERT_BLOCK
    block_expert = jnp.minimum(jnp.searchsorted(pad_end, block_start, side='right'), N_EXPERTS - 1)
    x_pad = jnp.concatenate([xt, jnp.zeros((1, d), xt.dtype)], axis=0)
    xb = x_pad[row_tok].reshape(n_blocks, EXPERT_BLOCK, d)

    def expert_block(args):
        xblk, e = args
        h = xblk @ w_gate_up[e] + b_gate_up[e]
        return clamped_swiglu(h) @ w_down[e] + b_down[e]

    yb = lax.map(expert_block, (xb, block_expert))
    y = jnp.zeros((n_tok + 1, d), jnp.float32).at[row_tok].add(
        yb.reshape(n_rows, d).astype(jnp.float32) * row_gate[:, None])
    return y[:n_tok].astype(x.dtype).reshape(bsz, seq, d)


def setup_inputs(seed: int = 0) -> dict:
    key = jax.random.key(seed)
    ks = jax.random.split(key, 24)
    n_a, n_b, n_c = _layers_of_kind(0), _layers_of_kind(1), _layers_of_kind(2)
    d = D_MODEL
    qkv_w = (N_Q_HEADS + 2 * N_KV_HEADS) * HEAD_DIM

    def nrm(k, shape, scale):
        return jax.random.normal(k, shape, jnp.float32) * scale

    return {
        "x": nrm(ks[0], (BATCH, SEQ, d), 1.0),
        "attn_w_qkv": nrm(ks[1], (n_a, d, qkv_w), d ** -0.5),
        "attn_q_norm": 1.0 + nrm(ks[2], (n_a, HEAD_DIM), 0.02),
        "attn_k_norm": 1.0 + nrm(ks[3], (n_a, HEAD_DIM), 0.02),
        "attn_w_o": nrm(ks[4], (n_a, N_Q_HEADS * HEAD_DIM, d), (N_Q_HEADS * HEAD_DIM) ** -0.5 * DEEPNORM_BETA),
        "gmlp_w_in": nrm(ks[5], (n_b, d, 2 * GMLP_WIDTH), d ** -0.5),
        "gmlp_norm_g": 1.0 + nrm(ks[6], (n_b, GMLP_WIDTH), 0.02),
        "gmlp_norm_b": nrm(ks[7], (n_b, GMLP_WIDTH), 0.02),
        "gmlp_w_s": nrm(ks[8], (n_b, GMLP_GROUPS, GMLP_CHUNK, GMLP_CHUNK), GMLP_CHUNK ** -0.5),
        "gmlp_b_s": 1.0 + nrm(ks[9], (n_b, GMLP_GROUPS, GMLP_CHUNK), 0.02),
        "gmlp_w_out": nrm(ks[10], (n_b, GMLP_WIDTH, d), GMLP_WIDTH ** -0.5 * DEEPNORM_BETA),
        "conv_w_in": nrm(ks[11], (n_c, d, 3 * d), d ** -0.5),
        "conv_w": nrm(ks[12], (n_c, CONV_WIDTH, d), CONV_WIDTH ** -0.5),
        "conv_w_out": nrm(ks[13], (n_c, d, d), d ** -0.5 * DEEPNORM_BETA),
        "ln_mix_g": 1.0 + nrm(ks[14], (DEPTH, d), 0.02),
        "ln_mix_b": nrm(ks[15], (DEPTH, d), 0.02),
        "ln_ffn_g": 1.0 + nrm(ks[16], (DEPTH, d), 0.02),
        "ln_ffn_b": nrm(ks[17], (DEPTH, d), 0.02),
        "router_w": nrm(ks[18], (DEPTH, d, N_EXPERTS), d ** -0.5),
        "router_b": nrm(ks[19], (DEPTH, N_EXPERTS), 0.01),
        "expert_w_gate_up": nrm(ks[20], (DEPTH, N_EXPERTS, d, 2 * D_FF), d ** -0.5),
        "expert_b_gate_up": nrm(ks[21], (DEPTH, N_EXPERTS, 2 * D_FF), 0.02),
        "expert_w_down": nrm(ks[22], (DEPTH, N_EXPERTS, D_FF, d), D_FF ** -0.5 * DEEPNORM_BETA),
        "expert_b_down": nrm(ks[23], (DEPTH, N_EXPERTS, d), 0.02),
    }


def reference(x, attn_w_qkv, attn_q_norm, attn_k_norm, attn_w_o,
              gmlp_w_in, gmlp_norm_g, gmlp_norm_b, gmlp_w_s, gmlp_b_s, gmlp_w_out,
              conv_w_in, conv_w, conv_w_out,
              ln_mix_g, ln_mix_b, ln_ffn_g, ln_ffn_b,
              router_w, router_b, expert_w_gate_up, expert_b_gate_up,
              expert_w_down, expert_b_down):
    for i in range(DEPTH):
        kind = i % N_MIXERS
        j = i // N_MIXERS
        if kind == 0:
            h = attention_mixer(x, attn_w_qkv[j], attn_q_norm[j], attn_k_norm[j], attn_w_o[j])
        elif kind == 1:
            h = gmlp_mixer(x, gmlp_w_in[j], gmlp_norm_g[j], gmlp_norm_b[j],
                           gmlp_w_s[j], gmlp_b_s[j], gmlp_w_out[j])
        else:
            h = shortconv_mixer(x, conv_w_in[j], conv_w[j], conv_w_out[j])
        x = layer_norm(DEEPNORM_ALPHA * x + h, ln_mix_g[i], ln_mix_b[i])
        f = moe_ffn(x, router_w[i], router_b[i], expert_w_gate_up[i], expert_b_gate_up[i],
                    expert_w_down[i], expert_b_down[i])
        x = layer_norm(DEEPNORM_ALPHA * x + f, ln_ffn_g[i], ln_ffn_b[i])
    return x
```

```python
from contextlib import ExitStack
import math
import numpy as np
import concourse.bass as bass
import concourse.mybir as mybir
from concourse.bass_utils import run_bass_kernel_spmd

F32 = mybir.dt.float32
BF16 = mybir.dt.bfloat16
I32 = mybir.dt.int32
ALU = mybir.AluOpType
AF = mybir.ActivationFunctionType
AX = mybir.AxisListType

ENGS = ("pe", "act", "dve", "pool", "sp")

P = 128
D = 1024
SEQ = 4096
NT = SEQ // P
DEPTH = 4
NE = 32
CAPS = (768, 896, 896, 896)
CAPMAX = max(CAPS)
NSLOT = NE * CAPMAX
TRASH = NSLOT
NROWS = NSLOT + P
ALPHA = float((2 * DEPTH) ** 0.25)
LN_EPS = 1e-5
QK_EPS = 1e-6
KINDS = (0, 1, 2, 0)
HPERM = tuple(g * 8 + t * 4 + r for g in range(2) for r in range(4) for t in range(2))


class Buf:
    __slots__ = ("name", "w", "r")

    def __init__(self, name=""):
        self.name = name
        self.w = None
        self.r = {}


class Sched:
    def __init__(self, nc, stack, ndma=None):
        self.nc = nc
        ndma = ndma or {"sp": 36, "act": 12, "pool": 36}
        self.sems = []
        self.prog = {e: [] for e in ENGS}
        self.csem = {}
        self.ccnt = {e: 0 for e in ENGS}
        self.pending = {e: None for e in ENGS}
        for e in ENGS:
            self.csem[e] = len(self.sems)
            self.sems.append(stack.enter_context(nc.semaphore("c_" + e)))
        self.sem2eng = {v: k for k, v in self.csem.items()}
        self.dsem = {}
        self.drr = {}
        for q, n in ndma.items():
            self.dsem[q] = []
            self.drr[q] = 0
            for i in range(n):
                self.dsem[q].append(len(self.sems))
                self.sems.append(stack.enter_context(nc.semaphore("d_%s%d" % (q, i))))
        self.dcnt = [0] * len(self.sems)
        self.known = {e: {} for e in ENGS}
        self.nops = 0

    def _need(self, eng, deps, tok):
        if tok is None:
            return
        s, v = tok
        if eng == "pe" and s == self.csem["pe"]:
            return
        if deps.get(s, 0) < v:
            deps[s] = v

    def _collect(self, eng, reads, writes):
        deps = {}
        for b in reads:
            self._need(eng, deps, b.w)
        for b in writes:
            self._need(eng, deps, b.w)
            for s, v in b.r.items():
                self._need(eng, deps, (s, v))
        out = []
        kn = self.known[eng]
        for s, v in deps.items():
            if kn.get(s, 0) >= v:
                continue
            e2 = self.sem2eng.get(s)
            if e2 is not None and v == self.ccnt[e2] + 1:
                p = self.pending[e2]
                assert p is not None
                p["inc"] = (s, 1)
                self.ccnt[e2] += 1
                self.pending[e2] = None
            kn[s] = v
            out.append((s, v))
        return out

    def _mark(self, tok, reads, writes):
        s, v = tok
        for b in reads:
            if b.r.get(s, 0) < v:
                b.r[s] = v
        for b in writes:
            b.w = tok
            b.r = {}

    def op(self, eng, fn, reads=(), writes=(), sig=False):
        waits = self._collect(eng, reads, writes)
        rec = {"waits": waits, "fn": fn, "inc": None}
        self.prog[eng].append(rec)
        if sig:
            self.ccnt[eng] += 1
            rec["inc"] = (self.csem[eng], 1)
            self.pending[eng] = None
            tok = (self.csem[eng], self.ccnt[eng])
        else:
            self.pending[eng] = rec
            tok = (self.csem[eng], self.ccnt[eng] + 1)
        self._mark(tok, reads, writes)
        self.nops += 1
        return tok

    def dma(self, q, fn, reads=(), writes=()):
        j = self.dsem[q][self.drr[q] % len(self.dsem[q])]
        self.drr[q] += 1
        waits = self._collect(q, reads, writes)
        prev = self.dcnt[j]
        if prev > 0 and self.known[q].get(j, 0) < prev:
            self.known[q][j] = prev
            waits.append((j, prev))
        self.dcnt[j] = prev + 16
        rec = {"waits": waits, "fn": fn, "inc": (j, 16)}
        self.prog[q].append(rec)
        tok = (j, prev + 16)
        self._mark(tok, reads, writes)
        self.nops += 1
        return tok

    def barrier(self):
        for e in ENGS:
            p = self.pending[e]
            if p is not None:
                p["inc"] = (self.csem[e], 1)
                self.ccnt[e] += 1
                self.pending[e] = None
        waits = []
        kn = self.known["sp"]
        for e in ENGS:
            if e == "sp":
                continue
            s, v = self.csem[e], self.ccnt[e]
            if v > 0 and kn.get(s, 0) < v:
                kn[s] = v
                waits.append((s, v))
        for q in self.dsem:
            for j in self.dsem[q]:
                v = self.dcnt[j]
                if v > 0 and kn.get(j, 0) < v:
                    kn[j] = v
                    waits.append((j, v))
        ssp = self.csem["sp"]
        self.ccnt["sp"] += 1
        val = self.ccnt["sp"]
        sem = self.sems[ssp]
        self.prog["sp"].append({"waits": waits, "fn": (lambda e: e.sem_inc(sem, 1)), "inc": None})
        for e in ENGS:
            if e == "sp":
                continue
            self.prog[e].append({"waits": [(ssp, val)], "fn": None, "inc": None})
        for e in ENGS:
            for e2 in ENGS:
                self.known[e][self.csem[e2]] = self.ccnt[e2]
            for q in self.dsem:
                for j in self.dsem[q]:
                    self.known[e][j] = self.dcnt[j]

    def emit(self):
        nc = self.nc
        self.barrier()
        sems = self.sems
        prog = self.prog

        def run(engine, lst):
            for rec in lst:
                for s, v in rec["waits"]:
                    engine.wait_ge(sems[s], v)
                if rec["fn"] is not None:
                    ins = rec["fn"](engine)
                    if rec["inc"] is not None:
                        ins.then_inc(sems[rec["inc"][0]], rec["inc"][1])

        with nc.Block() as block:
            @block.tensor
            def _(e):
                run(e, prog["pe"])

            @block.scalar
            def _(e):
                run(e, prog["act"])

            @block.vector
            def _(e):
                run(e, prog["dve"])

            @block.gpsimd
            def _(e):
                if getattr(self, "pool_init", None):
                    self.pool_init(e)
                run(e, prog["pool"])

            @block.sync
            def _(e):
                run(e, prog["sp"])


ARENA_BYTES = 204 * 1024


class Ctx:
    pass


def _isz(dt):
    return 2 if dt == BF16 else 4


def build(kinds=KINDS, debug=False):
    nc = bass.Bass("TRN2", target_bir_lowering=False)
    c = Ctx()
    c.nc = nc
    n_a = sum(1 for k in kinds if k == 0)
    n_b = sum(1 for k in kinds if k == 1)
    n_c = sum(1 for k in kinds if k == 2)
    NL = len(kinds)

    def din(name, shape, dt=F32):
        return nc.dram_tensor(name, list(shape), dt, kind="ExternalInput").ap()

    def dscr(name, shape, dt):
        return nc.dram_tensor(name, list(shape), dt, kind="Internal").ap()

    I = {}
    I["x"] = din("x", [SEQ, D])
    if n_a:
        I["wqkv"] = din("wqkv", [n_a, D, 1536])
        I["qn"] = din("qn", [n_a, 64])
        I["kn"] = din("kn", [n_a, 64])
        I["wo"] = din("wo", [n_a, D, D])
    if n_b:
        I["g_win"] = din("g_win", [n_b, D, 4096])
        I["g_ng"] = din("g_ng", [n_b, 2048])
        I["g_nb"] = din("g_nb", [n_b, 2048])
        I["g_wsT"] = din("g_wsT", [n_b, P, 8, P])
        I["g_bs"] = din("g_bs", [n_b, P, 8])
        I["g_wout"] = din("g_wout", [n_b, 2048, D])
    if n_c:
        I["c_win"] = din("c_win", [n_c, D, 3072])
        I["c_w"] = din("c_w", [n_c, P, 8, 3])
        I["c_wout"] = din("c_wout", [n_c, D, D])
    for nm in ("ln_mix_g", "ln_mix_b", "ln_ffn_g", "ln_ffn_b"):
        I[nm] = din(nm, [NL, D])
    I["router_w"] = din("router_w", [NL, D, NE])
    I["router_b"] = din("router_b", [NL, NE])
    I["e_wg"] = din("e_wg", [NL, NE, D, D])
    I["e_wl"] = din("e_wl", [NL, NE, D, D])
    I["e_bg"] = din("e_bg", [NL, P, NE, 8])
    I["e_bl"] = din("e_bl", [NL, P, NE, 8])
    I["e_wd"] = din("e_wd", [NL, NE, D, D])
    I["e_bd"] = din("e_bd", [NL, NE, D])
    I["c_ident"] = din("c_ident", [P, P])
    I["c_tri"] = din("c_tri", [P, P])
    I["c_cos"] = din("c_cos", [SEQ, 32])
    I["c_sin"] = din("c_sin", [SEQ, 32])
    I["c_ecap"] = din("c_ecap", [NL, P, NE])
    out = nc.dram_tensor("out", [SEQ, D], F32, kind="ExternalOutput").ap()

    Xs = dscr("Xs", [SEQ, D], F32)
    XTd = dscr("XTd", [NT, P, 8 * P], BF16)
    xs = dscr("xs_rows", [NROWS, D], BF16)
    ys = dscr("ys_rows", [NROWS, D], F32)

    with ExitStack() as st:
        S = Sched(nc, st)
        c.S = S
        regh = {}

        def _pool_init(e):
            regh["r"] = e.alloc_register("bc")
            e.reg_mov(regh["r"], NROWS - 1)
        S.pool_init = _pool_init
        A = st.enter_context(nc.sbuf_tensor("arena", [P, ARENA_BYTES // 4], F32))
        PSUM = st.enter_context(nc.psum_tensor("psum", [P, 4096], F32))
        pb = [Buf("bank%d" % k) for k in range(8)]

        def bank(k, n=1):
            return PSUM[:, k * 512:(k + n) * 512]

        state = {"off": 0}

        def T(shape, dt=F32):
            n = 1
            for s_ in shape[1:]:
                n *= s_
            cols = (n * _isz(dt) + 3) // 4
            off = state["off"]
            state["off"] = off + cols
            assert state["off"] * 4 <= ARENA_BYTES, "SBUF arena overflow %d" % (state["off"] * 4)
            ap = A[0:shape[0], off:off + cols]
            if dt != F32:
                ap = ap.bitcast(dt)
                if n % 2:
                    ap = ap[:, 0:n]
            if len(shape) == 3:
                ap = ap.rearrange("p (a b) -> p a b", b=shape[2])
            elif len(shape) == 4:
                ap = ap.rearrange("p (a b c) -> p a b c", b=shape[2], c=shape[3])
            return ap

        def TB(shape, dt=F32, name=""):
            return T(shape, dt), Buf(name)

        identf, b_identf = TB([P, P])
        identb, b_identb = TB([P, P], BF16)
        trib, b_trib = TB([P, P], BF16)
        onesb, b_onesb = TB([P, P], BF16)
        onesf, b_onesf = TB([P, 64])
        eps5, b_eps5 = TB([P, 1])
        eps6, b_eps6 = TB([P, 1])
        ecap, b_ecap = TB([P, NE])
        cosT, b_cos = TB([P, NT, 32])
        sinT, b_sin = TB([P, NT, 32])
        GATE, b_gate = TB([P, NT, 4])
        DEST, b_dest = TB([P, NT * 4], I32)
        cum = [TB([P, NE]), TB([P, NE])]
        tmpc, b_tmpc = TB([P, P])
        bconst = Buf("const")

        S.dma("sp", lambda e: e.dma_start(out=identf, in_=I["c_ident"]), writes=[b_identf])
        S.dma("sp", lambda e: e.dma_start(out=tmpc, in_=I["c_tri"]), writes=[b_tmpc])
        S.op("dve", lambda e: e.tensor_copy(identb, identf), reads=[b_identf], writes=[b_identb])
        S.op("dve", lambda e: e.tensor_copy(trib, tmpc), reads=[b_tmpc], writes=[b_trib])
        S.op("pool", lambda e: e.memset(onesb, 1.0), writes=[b_onesb])
        S.op("pool", lambda e: e.memset(onesf, 1.0), writes=[b_onesf])
        S.op("pool", lambda e: e.memset(eps5, LN_EPS), writes=[b_eps5])
        S.op("pool", lambda e: e.memset(eps6, QK_EPS), writes=[b_eps6])
        S.dma("sp", lambda e: e.dma_start(out=cosT, in_=I["c_cos"].rearrange("(i p) j -> p i j", p=P)), writes=[b_cos])
        S.dma("sp", lambda e: e.dma_start(out=sinT, in_=I["c_sin"].rearrange("(i p) j -> p i j", p=P)), writes=[b_sin])
        persist_off = state["off"]
        zt, b_zt = TB([P, D])
        S.op("pool", lambda e: e.memset(zt, 0.0), writes=[b_zt])
        S.dma("sp", lambda e: e.dma_start(out=ys[NSLOT:NROWS, :], in_=zt), reads=[b_zt])
        S.barrier()

        def load_w_bf16(dst, src2d, kc, bufs):
            for k in range(kc):
                S.dma("pool", lambda e, k=k: e.dma_start(out=dst[:, k, :], in_=src2d[k * P:(k + 1) * P, :]),
                      writes=bufs)

        def transposes_to_xtd(i, src_bf, b_src, psb_k, xt_sb, b_xt):
            ptb = bank(psb_k).bitcast(BF16).rearrange("p (a b) -> p a b", b=P)
            for cc in range(8):
                S.op("pe", lambda e, cc=cc: e.transpose(ptb[:, cc, :], src_bf[:, cc * P:(cc + 1) * P], identb),
                     reads=[b_src, b_identb], writes=[pb[psb_k]])
            S.op("dve", lambda e: e.tensor_copy(xt_sb, ptb), reads=[pb[psb_k]], writes=[b_xt])
            S.dma("sp", lambda e: e.dma_start(out=XTd[i], in_=xt_sb.rearrange("p a b -> p (a b)")), reads=[b_xt])

        def alloc_finish(mode, share=None):
            f = Ctx()
            f.psb = 0
            f.xr, f.b_xr = TB([P, D])
            f.v, f.b_v = TB([P, D])
            f.w, f.b_w = TB([P, D])
            f.st, f.b_st = TB([P, 2, 6])
            f.mv, f.b_mv = TB([P, 2])
            f.rs, f.b_rs = TB([P, 1])
            if share is None:
                f.lng, f.b_lng = TB([P, D])
                f.lnb, f.b_lnb = TB([P, D])
            else:
                f.lng, f.b_lng, f.lnb, f.b_lnb = share.lng, share.b_lng, share.lnb, share.b_lnb
                f.psb = 1
            f.ybf, f.b_ybf = TB([P, D], BF16)
            if mode == "mix":
                f.yT, f.b_yT = TB([P, 8, P])
                f.wr, f.b_wr = TB([P, 8, NE])
                f.brt, f.b_brt = TB([P, NE])
                f.lg, f.b_lg = TB([P, NE])
                f.t8, f.b_t8 = TB([P, 8])
                f.maskb, f.b_maskb = TB([P, NE], BF16)
                f.nm, f.b_nm = TB([P, 1])
                f.e4, f.b_e4 = TB([P, 4])
                f.s4, f.b_s4 = TB([P, 1])
                f.posf, f.b_posf = TB([P, NE])
                f.valid, f.b_valid = TB([P, NE])
                f.d2, f.b_d2 = TB([P, NE])
                f.oh, f.b_oh = TB([P, NE])
                f.destf, f.b_destf = TB([P, 4])
            else:
                f.xt_sb, f.b_xt = TB([P, 8, P], BF16)
            return f

        def finish_setup(f, L, mode):
            gname, bname = ("ln_mix_g", "ln_mix_b") if mode == "mix" else ("ln_ffn_g", "ln_ffn_b")
            S.dma("sp", lambda e: e.dma_start(out=f.lng, in_=I[gname][L].partition_broadcast(P)), writes=[f.b_lng])
            S.dma("sp", lambda e: e.dma_start(out=f.lnb, in_=I[bname][L].partition_broadcast(P)), writes=[f.b_lnb])
            if mode == "mix":
                S.dma("sp", lambda e: e.dma_start(out=f.wr, in_=I["router_w"][L].rearrange("(c p) n -> p c n", p=P)),
                      writes=[f.b_wr])
                S.dma("sp", lambda e: e.dma_start(out=f.brt, in_=I["router_b"][L].partition_broadcast(P)),
                      writes=[f.b_brt])
                S.op("pool", lambda e: e.memset(cum[0][0], 0.0), writes=[cum[0][1]])
                f.cap = CAPS[L % len(CAPS)]
                S.dma("sp", lambda e: e.dma_start(out=ecap, in_=I["c_ecap"][L]), writes=[b_ecap])

        def finish(f, i, hsrc, hbufs, mode, Xin, Xout, make_xt=True):
            rows = slice(i * P, (i + 1) * P)
            xr, v, w = f.xr, f.v, f.w
            S.dma("sp", lambda e: e.dma_start(out=xr, in_=Xin[rows, :]), writes=[f.b_xr])
            S.op("dve", lambda e: e.scalar_tensor_tensor(v, xr, ALPHA, hsrc, ALU.mult, ALU.add),
                 reads=[f.b_xr] + hbufs, writes=[f.b_v])
            for cc in range(2):
                S.op("dve", lambda e, cc=cc: e.bn_stats(f.st[:, cc, :], v[:, cc * 512:(cc + 1) * 512]),
                     reads=[f.b_v], writes=[f.b_st])
            S.op("dve", lambda e: e.bn_aggr(f.mv, f.st.rearrange("p a b -> p (a b)")), reads=[f.b_st], writes=[f.b_mv])
            S.op("act", lambda e: e.activation(f.rs, f.mv[:, 1:2], AF.Sqrt, bias=eps5[:, 0:1], scale=1.0),
                 reads=[f.b_mv, b_eps5], writes=[f.b_rs])
            S.op("dve", lambda e: e.reciprocal(f.rs, f.rs), reads=[f.b_rs], writes=[f.b_rs])
            S.op("dve", lambda e: e.tensor_scalar(w, v, f.mv[:, 0:1], f.rs[:, 0:1], ALU.subtract, ALU.mult),
                 reads=[f.b_v, f.b_mv, f.b_rs], writes=[f.b_w])
            S.op("dve", lambda e: e.tensor_tensor(v, w, f.lng, ALU.mult), reads=[f.b_w, f.b_lng], writes=[f.b_v])
            S.op("dve", lambda e: e.tensor_tensor(w, v, f.lnb, ALU.add), reads=[f.b_v, f.b_lnb], writes=[f.b_w])
            S.dma("sp", lambda e: e.dma_start(out=Xout[rows, :], in_=w), reads=[f.b_w])
            if mode == "ffn":
                if make_xt:
                    S.op("act", lambda e: e.copy(f.ybf, w), reads=[f.b_w], writes=[f.b_ybf])
                    transposes_to_xtd(i, f.ybf, f.b_ybf, f.psb, f.xt_sb, f.b_xt)
                return
            S.op("act", lambda e: e.copy(f.ybf, w), reads=[f.b_w], writes=[f.b_ybf])
            ptf = bank(0, 2).rearrange("p (a b) -> p a b", b=P)
            for cc in range(8):
                S.op("pe", lambda e, cc=cc: e.transpose(ptf[:, cc, :], w[:, cc * P:(cc + 1) * P], identf),
                     reads=[f.b_w, b_identf], writes=[pb[0], pb[1]])
            S.op("dve", lambda e: e.tensor_copy(f.yT, ptf), reads=[pb[0], pb[1]], writes=[f.b_yT])
            plg = bank(2)[:, 0:NE]
            for cc in range(8):
                S.op("pe", lambda e, cc=cc: e.matmul(plg, f.yT[:, cc, :], f.wr[:, cc, :], start=(cc == 0), stop=(cc == 7)),
                     reads=[f.b_yT, f.b_wr], writes=[pb[2]])
            S.op("dve", lambda e: e.tensor_tensor(f.lg, plg, f.brt, ALU.add), reads=[pb[2], f.b_brt], writes=[f.b_lg])
            S.op("dve", lambda e: e.max(f.t8, f.lg), reads=[f.b_lg], writes=[f.b_t8])
            S.op("dve", lambda e: e.tensor_single_scalar(f.maskb, f.lg, f.t8[:, 3:4], ALU.is_ge),
                 reads=[f.b_lg, f.b_t8], writes=[f.b_maskb])
            S.op("dve", lambda e: e.tensor_scalar_mul(f.nm, f.t8[:, 0:1], -1.0), reads=[f.b_t8], writes=[f.b_nm])
            S.op("act", lambda e: e.activation(f.e4, f.t8[:, 0:4], AF.Exp, bias=f.nm[:, 0:1], scale=1.0),
                 reads=[f.b_t8, f.b_nm], writes=[f.b_e4])
            S.op("dve", lambda e: e.reduce_sum(f.s4, f.e4, axis=AX.X), reads=[f.b_e4], writes=[f.b_s4])
            S.op("dve", lambda e: e.reciprocal(f.s4, f.s4), reads=[f.b_s4], writes=[f.b_s4])
            S.op("dve", lambda e: e.tensor_scalar_mul(GATE[:, i, :], f.e4, f.s4[:, 0:1]),
                 reads=[f.b_e4, f.b_s4], writes=[b_gate])
            ppos = bank(3)
            S.op("pe", lambda e: e.matmul(ppos[:, 0:NE], trib, f.maskb, start=True, stop=True),
                 reads=[b_trib, f.b_maskb], writes=[pb[3]])
            S.op("pe", lambda e: e.matmul(ppos[:, 64:64 + NE], onesb, f.maskb, start=True, stop=True),
                 reads=[b_onesb, f.b_maskb], writes=[pb[3]])
            cur, b_cur = cum[i % 2]
            nxt, b_nxt = cum[(i + 1) % 2]
            S.op("dve", lambda e: e.tensor_tensor(f.posf, ppos[:, 0:NE], cur, ALU.add), reads=[pb[3], b_cur],
                 writes=[f.b_posf])
            S.op("dve", lambda e: e.tensor_tensor(nxt, ppos[:, 64:64 + NE], cur, ALU.add), reads=[pb[3], b_cur],
                 writes=[b_nxt])
            S.op("dve", lambda e: e.tensor_single_scalar(f.valid, f.posf, float(f.cap), ALU.is_lt),
                 reads=[f.b_posf], writes=[f.b_valid])
            S.op("dve", lambda e: e.tensor_tensor(f.d2, f.posf, ecap, ALU.add), reads=[f.b_posf, b_ecap],
                 writes=[f.b_d2])
            S.op("dve", lambda e: e.tensor_tensor(f.posf, f.d2, f.valid, ALU.mult), reads=[f.b_d2, f.b_valid],
                 writes=[f.b_posf])
            S.op("dve", lambda e: e.tensor_scalar_add(f.d2, f.posf, float(TRASH)), reads=[f.b_posf], writes=[f.b_d2])
            for k in range(4):
                S.op("dve", lambda e, k=k: e.tensor_single_scalar(f.oh, f.lg, f.t8[:, k:k + 1], ALU.is_equal),
                     reads=[f.b_lg, f.b_t8], writes=[f.b_oh])
                S.op("dve", lambda e: e.tensor_tensor(f.valid, f.oh, f.d2, ALU.mult), reads=[f.b_oh, f.b_d2],
                     writes=[f.b_valid])
                S.op("dve", lambda e, k=k: e.reduce_sum(f.destf[:, k:k + 1], f.valid, axis=AX.X),
                     reads=[f.b_valid], writes=[f.b_destf])
            S.op("dve", lambda e: e.tensor_copy(DEST[:, i * 4:i * 4 + 4], f.destf), reads=[f.b_destf], writes=[b_dest])
            for k in range(4):
                S.dma("pool", lambda e, k=k: e.indirect_dma_start(
                    out=xs, out_offset=bass.IndirectOffsetOnAxis(ap=DEST[:, i * 4 + k:i * 4 + k + 1], axis=0),
                    in_=f.ybf, in_offset=None, bounds_check=regh["r"], oob_is_err=False),
                    reads=[f.b_ybf, b_dest])

        def phase_init():
            state["off"] = persist_off
            xin = [TB([P, D]) for _ in range(2)]
            xbf = [TB([P, D], BF16) for _ in range(2)]
            xt_sb = [TB([P, 8, P], BF16) for _ in range(2)]
            for i in range(NT):
                k = i % 2
                S.dma("sp", lambda e, i=i, k=k: e.dma_start(out=xin[k][0], in_=I["x"][i * P:(i + 1) * P, :]),
                      writes=[xin[k][1]])
                S.op("act", lambda e, k=k: e.copy(xbf[k][0], xin[k][0]), reads=[xin[k][1]], writes=[xbf[k][1]])
                transposes_to_xtd(i, xbf[k][0], xbf[k][1], k, xt_sb[k][0], xt_sb[k][1])
            S.barrier()

        def phase_conv(L, j, Xin):
            state["off"] = persist_off
            f = alloc_finish("mix")
            GT, b_GT = TB([P, 8, SEQ], BF16)
            wout, b_wout = TB([P, 8, D], BF16)
            win = [TB([P, 8, 3 * P], BF16) for _ in range(2)]
            cw, b_cw = TB([P, 8, 3])
            cx, b_cx = TB([P, SEQ + 2])
            bg_, b_bg = TB([P, SEQ])
            xtg = [TB([P, 8, 512], BF16) for _ in range(2)]
            ctmp = [TB([P, 512]) for _ in range(2)]
            yck = [TB([P, 512]) for _ in range(2)]
            finish_setup(f, L, "mix")
            S.dma("sp", lambda e: e.dma_start(out=cw, in_=I["c_w"][j]), writes=[b_cw])
            load_w_bf16(wout, I["c_wout"][j], 8, [b_wout])
            S.op("pool", lambda e: e.memset(cx[:, 0:1], 0.0), writes=[b_cx])
            S.op("pool", lambda e: e.memset(cx[:, SEQ + 1:SEQ + 2], 0.0), writes=[b_cx])
            gcnt = 0
            for fc in range(8):
                wk, b_wk = win[fc % 2]
                for part in range(3):
                    for k in range(8):
                        S.dma("pool", lambda e, k=k, part=part, fc=fc, wk=wk: e.dma_start(
                            out=wk[:, k, part * P:(part + 1) * P],
                            in_=I["c_win"][j][k * P:(k + 1) * P, part * D + fc * P: part * D + (fc + 1) * P]),
                            writes=[b_wk])
                for tg in range(8):
                    XT, b_XT = xtg[gcnt % 2]
                    gcnt += 1
                    for t in range(4):
                        S.dma("sp", lambda e, t=t, tg=tg, XT=XT: e.dma_start(
                            out=XT[:, :, t * P:(t + 1) * P], in_=XTd[tg * 4 + t].rearrange("p (a b) -> p a b", b=P)),
                            writes=[b_XT])
                    bks = (6, 7, 3)
                    for part in range(3):
                        for k in range(8):
                            S.op("pe", lambda e, k=k, part=part, wk=wk, XT=XT, bks=bks: e.matmul(
                                bank(bks[part]), wk[:, k, part * P:(part + 1) * P], XT[:, k, :],
                                start=(k == 0), stop=(k == 7)),
                                reads=[b_wk, b_XT], writes=[pb[bks[part]]])
                    tok = slice(tg * 512, (tg + 1) * 512)
                    ct, b_ct = ctmp[tg % 2]
                    S.op("act", lambda e, tok=tok: e.copy(bg_[:, tok], bank(6)), reads=[pb[6]], writes=[b_bg])
                    S.op("act", lambda e, ct=ct: e.copy(ct, bank(7)), reads=[pb[7]], writes=[b_ct])
                    S.op("dve", lambda e, ct=ct, tg=tg: e.tensor_tensor(cx[:, 1 + tg * 512:1 + (tg + 1) * 512], bank(3), ct, ALU.mult),
                         reads=[pb[3], b_ct], writes=[b_cx])
                for tg in range(8):
                    yc, b_yc = yck[tg % 2]
                    o = tg * 512
                    S.op("dve", lambda e, fc=fc, o=o, yc=yc: e.tensor_scalar_mul(yc, cx[:, o + 1:o + 513], cw[:, fc, 1:2]),
                         reads=[b_cx, b_cw], writes=[b_yc])
                    S.op("dve", lambda e, fc=fc, o=o, yc=yc: e.scalar_tensor_tensor(yc, cx[:, o:o + 512], cw[:, fc, 0:1], yc, ALU.mult, ALU.add),
                         reads=[b_cx, b_cw, b_yc], writes=[b_yc])
                    S.op("dve", lambda e, fc=fc, o=o, yc=yc: e.scalar_tensor_tensor(yc, cx[:, o + 2:o + 514], cw[:, fc, 2:3], yc, ALU.mult, ALU.add),
                         reads=[b_cx, b_cw, b_yc], writes=[b_yc])
                    S.op("pool", lambda e, fc=fc, o=o, yc=yc: e.tensor_tensor(GT[:, fc, o:o + 512], bg_[:, o:o + 512], yc, ALU.mult),
                         reads=[b_bg, b_yc], writes=[b_GT])
            for i in range(NT):
                for nh in range(2):
                    for k in range(8):
                        S.op("pe", lambda e, i=i, nh=nh, k=k: e.matmul(
                            bank(4 + nh), GT[:, k, i * P:(i + 1) * P], wout[:, k, nh * 512:(nh + 1) * 512],
                            start=(k == 0), stop=(k == 7)), reads=[b_GT, b_wout], writes=[pb[4 + nh]])
                finish(f, i, bank(4, 2), [pb[4], pb[5]], "mix", Xin, Xs)
            S.barrier()

        def phase_attn(L, j, Xin):
            state["off"] = persist_off
            f = alloc_finish("mix")
            KT, b_KT = TB([P, 2, SEQ], BF16)
            VE, b_VE = TB([P, NT, 4, P], BF16)
            wqkv, b_wqkv = TB([P, 8, 1536], BF16)
            wo, b_wo = TB([64, 16, D], BF16)
            QT, b_QT = TB([P, 8, 512], BF16)
            OT, b_OT = TB([64, 16, 512], BF16)
            xt_t = [TB([P, 8, P], BF16) for _ in range(2)]
            gq, b_gq = TB([P, 64])
            gk, b_gk = TB([P, 64])
            sq, b_sq = TB([P, D])
            ss, b_ss = TB([P, 16])
            qn_, b_qn = TB([P, D])
            t1, b_t1 = TB([P, 512])
            t2, b_t2 = TB([P, 512])
            qr, b_qr = TB([P, D], BF16)
            vtmp, b_vtmp = TB([P, 256])
            Pt = [TB([P, 1024], BF16) for _ in range(4)]
            bc, b_bc = TB([64, 512])
            finish_setup(f, L, "mix")
            S.dma("sp", lambda e: e.dma_start(out=gq, in_=I["qn"][j].partition_broadcast(P)), writes=[b_gq])
            S.dma("sp", lambda e: e.dma_start(out=gk, in_=I["kn"][j].partition_broadcast(P)), writes=[b_gk])
            load_w_bf16(wqkv, I["wqkv"][j], 8, [b_wqkv])
            for h in range(16):
                S.dma("pool", lambda e, h=h: e.dma_start(out=wo[:, h, :], in_=I["wo"][j][h * 64:(h + 1) * 64, :]),
                      writes=[b_wo])
            S.op("pool", lambda e: e.memset(VE.rearrange("p a b c -> p (a b c)"), 1.0), writes=[b_VE])

            def rms_rope(src_ps, src_bufs, nh, gain, b_gain, i):
                W = nh * 64
                S.op("act", lambda e: e.activation(sq[:, 0:W], src_ps, AF.Square), reads=src_bufs, writes=[b_sq])
                S.op("dve", lambda e: e.tensor_reduce(ss[:, 0:nh], sq[:, 0:W].rearrange("p (h d) -> p h d", d=64),
                                                       axis=AX.X, op=ALU.add), reads=[b_sq], writes=[b_ss])
                S.op("act", lambda e: e.activation(ss[:, 0:nh], ss[:, 0:nh], AF.Sqrt, bias=eps6[:, 0:1], scale=1.0 / 64),
                     reads=[b_ss, b_eps6], writes=[b_ss])
                S.op("dve", lambda e: e.reciprocal(ss[:, 0:nh], ss[:, 0:nh]), reads=[b_ss], writes=[b_ss])
                S.op("dve", lambda e: e.tensor_tensor(
                    qn_[:, 0:W].rearrange("p (h d) -> p h d", d=64), src_ps.rearrange("p (h d) -> p h d", d=64),
                    ss[:, 0:nh].unsqueeze(2).to_broadcast([P, nh, 64]), ALU.mult),
                    reads=src_bufs + [b_ss], writes=[b_qn])
                S.op("pool", lambda e: e.tensor_tensor(
                    qn_[:, 0:W].rearrange("p (h d) -> p h d", d=64), qn_[:, 0:W].rearrange("p (h d) -> p h d", d=64),
                    gain.unsqueeze(1).to_broadcast([P, nh, 64]), ALU.mult), reads=[b_qn, b_gain], writes=[b_qn])
                x4 = qn_[:, 0:W].rearrange("p (h j t) -> p h j t", j=32, t=2)
                o4 = qr[:, 0:W].rearrange("p (h j t) -> p h j t", j=32, t=2)
                x0, x1 = x4[:, :, :, 0], x4[:, :, :, 1]
                Cb = cosT[:, i, :].unsqueeze(1).to_broadcast([P, nh, 32])
                Sb = sinT[:, i, :].unsqueeze(1).to_broadcast([P, nh, 32])
                a1 = t1[:, 0:nh * 32].rearrange("p (h j) -> p h j", j=32)
                a2 = t2[:, 0:nh * 32].rearrange("p (h j) -> p h j", j=32)
                S.op("dve", lambda e: e.tensor_tensor(a1, x0, Cb, ALU.mult), reads=[b_qn, b_cos], writes=[b_t1])
                S.op("pool", lambda e: e.tensor_tensor(a2, x1, Sb, ALU.mult), reads=[b_qn, b_sin], writes=[b_t2])
                S.op("dve", lambda e: e.tensor_tensor(o4[:, :, :, 0], a1, a2, ALU.subtract), reads=[b_t1, b_t2],
                     writes=[b_qr])
                S.op("dve", lambda e: e.tensor_tensor(a1, x0, Sb, ALU.mult), reads=[b_qn, b_sin, b_qr], writes=[b_t1])
                S.op("pool", lambda e: e.tensor_tensor(a2, x1, Cb, ALU.mult), reads=[b_qn, b_cos, b_qr], writes=[b_t2])
                S.op("dve", lambda e: e.tensor_tensor(o4[:, :, :, 1], a1, a2, ALU.add), reads=[b_t1, b_t2],
                     writes=[b_qr])

            def load_xt(i):
                xt, b_xt = xt_t[i % 2]
                S.dma("sp", lambda e: e.dma_start(out=xt, in_=XTd[i].rearrange("p (a b) -> p a b", b=P)), writes=[b_xt])
                return xt, b_xt

            for i in range(NT):
                xt, b_xt = load_xt(i)
                for k in range(8):
                    S.op("pe", lambda e, k=k, xt=xt: e.matmul(bank(6), xt[:, k, :], wqkv[:, k, 1024:1536],
                                                               start=(k == 0), stop=(k == 7)),
                         reads=[b_xt, b_wqkv], writes=[pb[6]])
                S.op("act", lambda e: e.copy(vtmp, bank(6)[:, 256:512]), reads=[pb[6]], writes=[b_vtmp])
                S.op("pool", lambda e, i=i: e.tensor_copy(VE[:, i, :, 0:64], vtmp.rearrange("p (a b) -> p a b", b=64)),
                     reads=[b_vtmp], writes=[b_VE])
                rms_rope(bank(6)[:, 0:256], [pb[6]], 4, gk, b_gk, i)
                ptb = bank(0).bitcast(BF16)
                for kp in range(2):
                    S.op("pe", lambda e, kp=kp: e.transpose(ptb[:, kp * P:(kp + 1) * P], qr[:, kp * P:(kp + 1) * P], identb),
                         reads=[b_qr, b_identb], writes=[pb[0]])
                S.op("dve", lambda e, i=i: e.tensor_copy(KT[:, :, i * P:(i + 1) * P],
                                                          ptb[:, 0:256].rearrange("p (a b) -> p a b", b=P)),
                     reads=[pb[0]], writes=[b_KT])
            pcount = 0
            for ch in range(8):
                for t in range(4):
                    i = ch * 4 + t
                    xt, b_xt = load_xt(i)
                    for nh in range(2):
                        for k in range(8):
                            S.op("pe", lambda e, k=k, nh=nh, xt=xt: e.matmul(
                                bank(4 + nh), xt[:, k, :], wqkv[:, k, nh * 512:(nh + 1) * 512],
                                start=(k == 0), stop=(k == 7)), reads=[b_xt, b_wqkv], writes=[pb[4 + nh]])
                    rms_rope(bank(4, 2), [pb[4], pb[5]], 16, gq, b_gq, i)
                    ptb = bank(0).bitcast(BF16)
                    for pr in range(8):
                        S.op("pe", lambda e, pr=pr, ptb=ptb: e.transpose(
                            ptb[:, pr * P:(pr + 1) * P], qr[:, pr * P:(pr + 1) * P], identb),
                            reads=[b_qr, b_identb], writes=[pb[0]])
                    S.op("dve", lambda e, t=t, ptb=ptb: e.tensor_copy(
                        QT[:, :, t * P:(t + 1) * P], ptb.rearrange("p (a b) -> p a b", b=P)),
                        reads=[pb[0]], writes=[b_QT])
                items = [(pr, st_) for pr in range(8) for st_ in range(NT)]
                SBK = (6, 4, 2)
                LA = 2
                deferred = []

                def emit_norm(pr, accs):
                    for half in range(2):
                        h = HPERM[pr * 2 + half]
                        ab = accs + half
                        S.op("dve", lambda e, ab=ab: e.reciprocal(bc, bank(ab)[64:128, :]), reads=[pb[ab]], writes=[b_bc])
                        S.op("dve", lambda e, ab=ab, h=h: e.tensor_tensor(OT[:, h, :], bank(ab)[0:64, :], bc, ALU.mult),
                             reads=[pb[ab], b_bc], writes=[b_OT])

                def emit_pv(n):
                    pr, st_ = items[n]
                    pt, b_pt = Pt[n % 4]
                    accs = 0
                    for half in range(2):
                        kv = (pr // 4) * 2 + half
                        S.op("pe", lambda e, kv=kv, st_=st_, pt=pt, half=half, accs=accs: e.matmul(
                            bank(accs + half), VE[:, st_, kv, :], pt[:, half * 512:(half + 1) * 512],
                            start=(st_ == 0), stop=(st_ == NT - 1)),
                            reads=[b_VE, b_pt], writes=[pb[accs + half]])
                    if st_ == NT - 1:
                        deferred.append([1, pr, accs])

                for n in range(len(items) + LA):
                    if n < len(items):
                        pr, st_ = items[n]
                        kp = pr // 4
                        sb0 = SBK[n % 3]
                        pt, b_pt = Pt[n % 4]
                        for half in range(2):
                            rows = slice(half * 64, (half + 1) * 64)
                            S.op("pe", lambda e, kp=kp, st_=st_, pr=pr, sb0=sb0, half=half, rows=rows: e.matmul(
                                bank(sb0 + half), KT[rows, kp, st_ * P:(st_ + 1) * P], QT[rows, pr, :], start=True, stop=True),
                                reads=[b_KT, b_QT], writes=[pb[sb0 + half]], sig=(half == 1))
                        S.op("act", lambda e, sb0=sb0, pt=pt: e.activation(pt, bank(sb0, 2), AF.Exp, scale=0.125),
                             reads=[pb[sb0], pb[sb0 + 1]], writes=[b_pt], sig=True)
                    if n >= LA:
                        emit_pv(n - LA)
                    for d in list(deferred):
                        d[0] -= 1
                        if d[0] <= 0:
                            emit_norm(d[1], d[2])
                            deferred.remove(d)
                for d in deferred:
                    emit_norm(d[1], d[2])
                for t in range(4):
                    i = ch * 4 + t
                    for nh in range(2):
                        for h in range(16):
                            S.op("pe", lambda e, h=h, nh=nh, t=t: e.matmul(
                                bank(4 + nh), OT[:, h, t * P:(t + 1) * P], wo[:, h, nh * 512:(nh + 1) * 512],
                                start=(h == 0), stop=(h == 15)), reads=[b_OT, b_wo], writes=[pb[4 + nh]])
                    finish(f, i, bank(4, 2), [pb[4], pb[5]], "mix", Xin, Xs)
            S.barrier()

        def phase_gmlp(L, j, Xin):
            state["off"] = persist_off
            f = alloc_finish("mix")
            win, b_win = TB([P, 8, 4096], BF16)
            wout, b_wout = TB([P, 16, D], BF16)
            wsT, b_wsT = TB([P, 8, P], BF16)
            wsf, b_wsf = TB([P, 8, P])
            bs, b_bs = TB([P, 8])
            ng, b_ng = TB([P, 2048])
            nb_, b_nb = TB([P, 2048])
            z, b_z = TB([P, 4096])
            vn, b_vn = TB([P, 2048])
            vnb, b_vnb = TB([P, 2048], BF16)
            gtd, b_gtd = TB([P, 2048], BF16)
            gT, b_gT = TB([P, 16, P], BF16)
            st4, b_st4 = TB([P, 4, 6])
            mv2, b_mv2 = TB([P, 2])
            rs2, b_rs2 = TB([P, 1])
            xt_t = [TB([P, 8, P], BF16) for _ in range(2)]
            finish_setup(f, L, "mix")
            load_w_bf16(win, I["g_win"][j], 8, [b_win])
            load_w_bf16(wout, I["g_wout"][j], 16, [b_wout])
            S.dma("sp", lambda e: e.dma_start(out=wsf, in_=I["g_wsT"][j]), writes=[b_wsf])
            S.op("dve", lambda e: e.tensor_copy(wsT, wsf), reads=[b_wsf], writes=[b_wsT])
            S.dma("sp", lambda e: e.dma_start(out=bs, in_=I["g_bs"][j]), writes=[b_bs])
            S.dma("sp", lambda e: e.dma_start(out=ng, in_=I["g_ng"][j].partition_broadcast(P)), writes=[b_ng])
            S.dma("sp", lambda e: e.dma_start(out=nb_, in_=I["g_nb"][j].partition_broadcast(P)), writes=[b_nb])
            for i in range(NT):
                xt, b_xt = xt_t[i % 2]
                S.dma("sp", lambda e, i=i, xt=xt: e.dma_start(out=xt, in_=XTd[i].rearrange("p (a b) -> p a b", b=P)),
                      writes=[b_xt])
                for cb in range(8):
                    bk = 6 + cb % 2
                    for k in range(8):
                        S.op("pe", lambda e, k=k, cb=cb, bk=bk, xt=xt: e.matmul(
                            bank(bk), xt[:, k, :], win[:, k, cb * 512:(cb + 1) * 512], start=(k == 0), stop=(k == 7)),
                            reads=[b_xt, b_win], writes=[pb[bk]])
                    S.op("act", lambda e, cb=cb, bk=bk: e.activation(z[:, cb * 512:(cb + 1) * 512], bank(bk), AF.Gelu),
                         reads=[pb[bk]], writes=[b_z])
                u = z[:, 0:2048]
                v = z[:, 2048:4096]
                for cc in range(4):
                    S.op("dve", lambda e, cc=cc: e.bn_stats(st4[:, cc, :], v[:, cc * 512:(cc + 1) * 512]),
                         reads=[b_z], writes=[b_st4])
                S.op("dve", lambda e: e.bn_aggr(mv2, st4.rearrange("p a b -> p (a b)")), reads=[b_st4], writes=[b_mv2])
                S.op("act", lambda e: e.activation(rs2, mv2[:, 1:2], AF.Sqrt, bias=eps5[:, 0:1], scale=1.0),
                     reads=[b_mv2, b_eps5], writes=[b_rs2])
                S.op("dve", lambda e: e.reciprocal(rs2, rs2), reads=[b_rs2], writes=[b_rs2])
                S.op("dve", lambda e: e.tensor_scalar(vn, v, mv2[:, 0:1], rs2[:, 0:1], ALU.subtract, ALU.mult),
                     reads=[b_z, b_mv2, b_rs2], writes=[b_vn])
                S.op("dve", lambda e: e.tensor_tensor(vn, vn, ng, ALU.mult), reads=[b_vn, b_ng], writes=[b_vn])
                S.op("dve", lambda e: e.tensor_tensor(vnb, vn, nb_, ALU.add), reads=[b_vn, b_nb], writes=[b_vnb])
                for g in range(8):
                    bk = g // 2
                    col = (g % 2) * 256
                    S.op("pe", lambda e, g=g, bk=bk, col=col: e.matmul(
                        bank(bk)[:, col:col + 256], wsT[:, g, :], vnb[:, g * 256:(g + 1) * 256], start=True, stop=True),
                        reads=[b_wsT, b_vnb], writes=[pb[bk]])
                for g in range(8):
                    bk = g // 2
                    col = (g % 2) * 256
                    S.op("dve", lambda e, g=g, bk=bk, col=col: e.scalar_tensor_tensor(
                        gtd[:, g * 256:(g + 1) * 256], bank(bk)[:, col:col + 256], bs[:, g:g + 1],
                        u[:, g * 256:(g + 1) * 256], ALU.add, ALU.mult), reads=[pb[bk], b_bs, b_z], writes=[b_gtd])
                ptb = bank(6, 2).bitcast(BF16).rearrange("p (a b) -> p a b", b=P)
                for fc in range(16):
                    S.op("pe", lambda e, fc=fc: e.transpose(ptb[:, fc, :], gtd[:, fc * P:(fc + 1) * P], identb),
                         reads=[b_gtd, b_identb], writes=[pb[6], pb[7]])
                S.op("act", lambda e: e.copy(gT, ptb), reads=[pb[6], pb[7]], writes=[b_gT])
                for nh in range(2):
                    for fc in range(16):
                        S.op("pe", lambda e, fc=fc, nh=nh: e.matmul(
                            bank(4 + nh), gT[:, fc, :], wout[:, fc, nh * 512:(nh + 1) * 512],
                            start=(fc == 0), stop=(fc == 15)), reads=[b_gT, b_wout], writes=[pb[4 + nh]])
                finish(f, i, bank(4, 2), [pb[4], pb[5]], "mix", Xin, Xs)
            S.barrier()

        def phase_experts(L):
            state["off"] = persist_off
            wg = [TB([P, 8, D], BF16) for _ in range(2)]
            wl = [TB([P, 8, D], BF16) for _ in range(2)]
            wd = [TB([P, 8, D], BF16) for _ in range(2)]
            bgT, b_bgT = TB([P, NE, 8])
            blT, b_blT = TB([P, NE, 8])
            CAP = CAPS[L % len(CAPS)]
            NB = CAP // P
            HC = 512
            CHUNKS = [(0, 512), (512, CAP - 512)]
            bdb = [TB([P, D]) for _ in range(2)]
            Xe = [TB([P, NB, D], BF16)] * 2
            XeT, b_XeT = TB([P, 8, CAP], BF16)
            actT, b_actT = TB([P, 8, CAP], BF16)
            gt_ = [TB([P, HC]) for _ in range(2)]
            sg = [TB([P, HC]) for _ in range(2)]
            lt = [TB([P, HC]) for _ in range(2)]
            l2 = [TB([P, HC]) for _ in range(2)]
            ysb = [TB([P, D]) for _ in range(2)]
            S.dma("sp", lambda e: e.dma_start(out=bgT, in_=I["e_bg"][L]), writes=[b_bgT])
            S.dma("sp", lambda e: e.dma_start(out=blT, in_=I["e_bl"][L]), writes=[b_blT])
            S.op("dve", lambda e: e.tensor_scalar_add(blT.rearrange("p a b -> p (a b)"), blT.rearrange("p a b -> p (a b)"), 1.0),
                 reads=[b_blT], writes=[b_blT])

            NSTG = 4
            stg = [TB([P, D]) for _ in range(NSTG)]
            wbuf = [[[Buf() for _ in range(8)] for _ in range(3)] for _ in range(2)]
            wten = [[wg[0][0], wl[0][0], wd[0][0]], [wg[1][0], wl[1][0], wd[1][0]]]
            wsrc = ["e_wg", "e_wl", "e_wd"]
            from collections import deque
            castq = deque()
            chunk_no = [0]

            def issue_chunk_dma(item):
                ex_, m_, kk_, slot = item
                sg_, b_sg = stg[slot]
                S.dma("sp", lambda e: e.dma_start(out=sg_, in_=I[wsrc[m_]][L, ex_][kk_ * P:(kk_ + 1) * P, :]),
                      writes=[b_sg])

            dmaq = deque()

            def load_expert(ex):
                k = ex % 2
                S.dma("sp", lambda e: e.dma_start(out=bdb[k][0], in_=I["e_bd"][L, ex].partition_broadcast(P)), writes=[bdb[k][1]])
                S.dma("sp", lambda e: e.dma_start(
                    out=Xe[k][0], in_=xs[ex * CAP:(ex + 1) * CAP, :].rearrange("(t p) d -> p t d", p=P)),
                    writes=[Xe[k][1]])
                for m_ in range(3):
                    for kk_ in range(8):
                        slot = chunk_no[0] % NSTG
                        chunk_no[0] += 1
                        item = (ex, m_, kk_, slot)
                        castq.append(item)
                        dmaq.append(item)
                while dmaq and (len(castq) - len(dmaq)) < NSTG:
                    issue_chunk_dma(dmaq.popleft())

            def do_casts(n):
                for _ in range(n):
                    if not castq:
                        return
                    ex_, m_, kk_, slot = castq.popleft()
                    k_ = ex_ % 2
                    sg_, b_sg = stg[slot]
                    dst = wten[k_][m_][:, kk_, :]
                    eng = "pool" if (kk_ % 4 == 3) else "act"
                    if eng == "act":
                        S.op("act", lambda e, dst=dst, sg_=sg_: e.copy(dst, sg_), reads=[b_sg], writes=[wbuf[k_][m_][kk_]])
                    else:
                        S.op("pool", lambda e, dst=dst, sg_=sg_: e.tensor_copy(dst, sg_), reads=[b_sg], writes=[wbuf[k_][m_][kk_]])
                    while dmaq and (len(castq) - len(dmaq)) < NSTG:
                        issue_chunk_dma(dmaq.popleft())

            load_expert(0)
            do_casts(24)
            cnt = 0
            ycnt = 0
            for ex in range(NE):
                k = ex % 2
                for tt in range(NB):
                    bk = tt % 2
                    ptb = bank(bk).bitcast(BF16).rearrange("p (a b) -> p a b", b=P)
                    for cc in range(8):
                        S.op("pe", lambda e, cc=cc, tt=tt, k=k, ptb=ptb: e.transpose(
                            ptb[:, cc, :], Xe[k][0][:, tt, cc * P:(cc + 1) * P], identb),
                            reads=[Xe[k][1], b_identb], writes=[pb[bk]])
                    S.op("dve", lambda e, tt=tt, ptb=ptb: e.tensor_copy(XeT[:, :, tt * P:(tt + 1) * P], ptb),
                         reads=[pb[bk]], writes=[b_XeT])
                if ex + 1 < NE:
                    load_expert(ex + 1)
                for fc in range(8):
                    for th in range(2):
                        gb_k = 2 + cnt % 2
                        lb_k = 4 + cnt % 2
                        m = cnt % 2
                        cnt += 1
                        tok = slice(CHUNKS[th][0], CHUNKS[th][0] + CHUNKS[th][1])
                        HW = CHUNKS[th][1]
                        for kk in range(8):
                            S.op("pe", lambda e, kk=kk, fc=fc, tok=tok, gb_k=gb_k, k=k, HW=HW: e.matmul(
                                bank(gb_k)[:, 0:HW], wg[k][0][:, kk, fc * P:(fc + 1) * P], XeT[:, kk, tok],
                                start=(kk == 0), stop=(kk == 7)), reads=[wbuf[k][0][kk], b_XeT], writes=[pb[gb_k]], sig=(kk == 7))
                        for kk in range(8):
                            S.op("pe", lambda e, kk=kk, fc=fc, tok=tok, lb_k=lb_k, k=k, HW=HW: e.matmul(
                                bank(lb_k)[:, 0:HW], wl[k][0][:, kk, fc * P:(fc + 1) * P], XeT[:, kk, tok],
                                start=(kk == 0), stop=(kk == 7)), reads=[wbuf[k][1][kk], b_XeT], writes=[pb[lb_k]], sig=(kk == 7))
                        S.op("dve", lambda e, gb_k=gb_k, m=m, ex=ex, fc=fc, HW=HW: e.tensor_scalar(
                            gt_[m][0][:, 0:HW], bank(gb_k)[:, 0:HW], bgT[:, ex, fc:fc + 1], 7.0, ALU.add, ALU.min),
                            reads=[pb[gb_k], b_bgT], writes=[gt_[m][1]])
                        S.op("act", lambda e, m=m, HW=HW: e.activation(sg[m][0][:, 0:HW], gt_[m][0][:, 0:HW], AF.Sigmoid, scale=1.702),
                             reads=[gt_[m][1]], writes=[sg[m][1]])
                        S.op("dve", lambda e, lb_k=lb_k, m=m, ex=ex, fc=fc, HW=HW: e.tensor_scalar(
                            lt[m][0][:, 0:HW], bank(lb_k)[:, 0:HW], blT[:, ex, fc:fc + 1], 8.0, ALU.add, ALU.min),
                            reads=[pb[lb_k], b_blT], writes=[lt[m][1]])
                        S.op("dve", lambda e, m=m, HW=HW: e.tensor_tensor(l2[m][0][:, 0:HW], sg[m][0][:, 0:HW], gt_[m][0][:, 0:HW], ALU.mult),
                             reads=[sg[m][1], gt_[m][1]], writes=[l2[m][1]])
                        S.op("dve", lambda e, m=m, fc=fc, tok=tok, HW=HW: e.scalar_tensor_tensor(
                            actT[:, fc, tok], lt[m][0][:, 0:HW], -6.0, l2[m][0][:, 0:HW], ALU.max, ALU.mult),
                            reads=[lt[m][1], l2[m][1]], writes=[b_actT])
                        do_casts(1)
                for tt in range(NB):
                    yk = ycnt % 2
                    ycnt += 1
                    for nh in range(2):
                        bk = 6 + nh
                        ncol = slice(nh * 512, (nh + 1) * 512)
                        for fc in range(8):
                            S.op("pe", lambda e, bk=bk, ncol=ncol, fc=fc, tt=tt, k=k: e.matmul(
                                bank(bk), actT[:, fc, tt * P:(tt + 1) * P], wd[k][0][:, fc, ncol],
                                start=(fc == 0), stop=(fc == 7)), reads=[b_actT, wbuf[k][2][fc]], writes=[pb[bk]], sig=(fc == 7))
                        S.op("dve", lambda e, bk=bk, ncol=ncol, yk=yk, k=k: e.tensor_tensor(
                            ysb[yk][0][:, ncol], bank(bk), bdb[k][0][:, ncol], ALU.add),
                            reads=[pb[bk], bdb[k][1]], writes=[ysb[yk][1]])
                    r0 = ex * CAP + tt * P
                    S.dma("act", lambda e, r0=r0, yk=yk: e.dma_start(out=ys[r0:r0 + P, :], in_=ysb[yk][0]),
                          reads=[ysb[yk][1]])
                    do_casts(2)
            S.barrier()

        def phase_combine(L, Xout, make_xt):
            state["off"] = persist_off
            f = alloc_finish("ffn")
            f2 = alloc_finish("ffn", share=f)
            R = [[TB([P, D]) for _ in range(4)] for _ in range(2)]
            acc = [TB([P, D]) for _ in range(2)]
            finish_setup(f, L, "ffn")
            for i in range(NT):
                k2 = i % 2
                for k in range(4):
                    S.dma("pool", lambda e, i=i, k=k, k2=k2: e.indirect_dma_start(
                        out=R[k2][k][0], out_offset=None, in_=ys,
                        in_offset=bass.IndirectOffsetOnAxis(ap=DEST[:, i * 4 + k:i * 4 + k + 1], axis=0),
                        bounds_check=regh["r"], oob_is_err=False), reads=[b_dest], writes=[R[k2][k][1]])
                a, b_a = acc[k2]
                S.op("dve", lambda e, i=i, k2=k2, a=a: e.tensor_scalar_mul(a, R[k2][0][0], GATE[:, i, 0:1]),
                     reads=[R[k2][0][1], b_gate], writes=[b_a])
                for k in range(1, 4):
                    eng = "dve"
                    S.op(eng, lambda e, i=i, k=k, k2=k2, a=a: e.scalar_tensor_tensor(
                        a, R[k2][k][0], GATE[:, i, k:k + 1], a, ALU.mult, ALU.add),
                        reads=[R[k2][k][1], b_gate, b_a], writes=[b_a])
                finish(f if i % 2 == 0 else f2, i, a, [b_a], "ffn", Xs, Xout, make_xt=make_xt)
            S.barrier()

        phase_init()
        ja = jb = jc = 0
        for L, kind in enumerate(kinds):
            Xin = I["x"] if L == 0 else Xs
            if kind == 0:
                phase_attn(L, ja, Xin)
                ja += 1
            elif kind == 1:
                phase_gmlp(L, jb, Xin)
                jb += 1
            else:
                phase_conv(L, jc, Xin)
                jc += 1
            phase_experts(L)
            last = (L == NL - 1)
            phase_combine(L, out if last else Xs, make_xt=not last)
        S.emit()
        c.nops = S.nops
    return nc


def _rope_tables():
    rows = SEQ // 64
    t = np.arange(rows * 64, dtype=np.int32)
    row = (t // 64).astype(np.float32)
    col = (t % 64).astype(np.float32)
    n_pairs = 16
    inv = (np.float32(10000.0) ** (-np.arange(n_pairs, dtype=np.float32) / np.float32(n_pairs))).astype(np.float32)
    ang = np.concatenate([row[:, None] * inv, col[:, None] * inv], -1).astype(np.float32)
    return np.cos(ang).astype(np.float32), np.sin(ang).astype(np.float32)


def prepare_shared(inp, kinds=KINDS):
    f = lambda a: np.ascontiguousarray(np.asarray(a, dtype=np.float32))
    sh = {}
    if any(k == 0 for k in kinds):
        wq = np.asarray(inp["attn_w_qkv"])
        qcols = np.concatenate([np.arange(h * 64, (h + 1) * 64) for h in HPERM])
        sh["wqkv"] = f(np.concatenate([wq[..., qcols], wq[..., 1024:]], axis=-1))
        sh["qn"] = f(inp["attn_q_norm"])
        sh["kn"] = f(inp["attn_k_norm"])
        sh["wo"] = f(inp["attn_w_o"])
    if any(k == 1 for k in kinds):
        sh["g_win"] = f(inp["gmlp_w_in"])
        sh["g_ng"] = f(inp["gmlp_norm_g"])
        sh["g_nb"] = f(inp["gmlp_norm_b"])
        sh["g_wsT"] = f(np.transpose(np.asarray(inp["gmlp_w_s"]), (0, 3, 1, 2)))
        sh["g_bs"] = f(np.transpose(np.asarray(inp["gmlp_b_s"]), (0, 2, 1)))
        sh["g_wout"] = f(inp["gmlp_w_out"])
    if any(k == 2 for k in kinds):
        sh["c_win"] = f(inp["conv_w_in"])
        cw = np.asarray(inp["conv_w"])
        sh["c_w"] = f(np.transpose(cw.reshape(cw.shape[0], 3, 8, P), (0, 3, 2, 1)))
        sh["c_wout"] = f(inp["conv_w_out"])
    for nm in ("ln_mix_g", "ln_mix_b", "ln_ffn_g", "ln_ffn_b", "router_w", "router_b"):
        sh[nm] = f(inp[nm])
    wgu = np.asarray(inp["expert_w_gate_up"])
    sh["e_wg"] = f(wgu[..., 0::2])
    sh["e_wl"] = f(wgu[..., 1::2])
    bgu = np.asarray(inp["expert_b_gate_up"])
    nl = bgu.shape[0]
    sh["e_bg"] = f(np.transpose(bgu[..., 0::2].reshape(nl, NE, 8, P), (0, 3, 1, 2)))
    sh["e_bl"] = f(np.transpose(bgu[..., 1::2].reshape(nl, NE, 8, P), (0, 3, 1, 2)))
    sh["e_wd"] = f(inp["expert_w_down"])
    sh["e_bd"] = f(inp["expert_b_down"])
    sh["c_ident"] = np.eye(P, dtype=np.float32)
    sh["c_tri"] = np.triu(np.ones((P, P), dtype=np.float32), 1)
    cs, sn = _rope_tables()
    sh["c_cos"] = cs
    sh["c_sin"] = sn
    sh["c_ecap"] = np.stack([np.broadcast_to((np.arange(NE, dtype=np.float32) * CAPS[l % len(CAPS)] - TRASH)[None, :], (P, NE))
                             for l in range(len(kinds))], 0).astype(np.float32).copy()
    return sh


_NC_CACHE = {}


def kernel(**inputs):
    x = np.asarray(inputs["x"], dtype=np.float32)
    B = x.shape[0]
    sh = prepare_shared(inputs)
    if "nc" not in _NC_CACHE:
        _NC_CACHE["nc"] = build()
    nc = _NC_CACHE["nc"]
    in_maps = []
    for b in range(B):
        m = dict(sh)
        m["x"] = np.ascontiguousarray(x[b])
        in_maps.append(m)
    res = run_bass_kernel_spmd(nc, in_maps, core_ids=list(range(B)))
    return np.stack([np.asarray(r["out"]) for r in res.results], axis=0).astype(np.float32)
```

```python
from contextlib import ExitStack
import math
import numpy as np
import concourse.bass as bass
import concourse.mybir as mybir
from concourse.bass_utils import run_bass_kernel_spmd

F32 = mybir.dt.float32
BF16 = mybir.dt.bfloat16
I32 = mybir.dt.int32
ALU = mybir.AluOpType
AF = mybir.ActivationFunctionType
AX = mybir.AxisListType

ENGS = ("pe", "act", "dve", "pool", "sp")

P = 128
D = 1024
SEQ = 4096
NT = SEQ // P
DEPTH = 4
NE = 32
CAPS = (768, 896, 896, 896)
CAPMAX = max(CAPS)
NSLOT = NE * CAPMAX
TRASH = NSLOT
NROWS = NSLOT + P
ALPHA = float((2 * DEPTH) ** 0.25)
LN_EPS = 1e-5
QK_EPS = 1e-6
KINDS = (0, 1, 2, 0)
HPERM = tuple(g * 8 + t * 4 + r for g in range(2) for r in range(4) for t in range(2))


class Buf:
    __slots__ = ("name", "w", "r")

    def __init__(self, name=""):
        self.name = name
        self.w = None
        self.r = {}


class Sched:
    def __init__(self, nc, stack, ndma=None):
        self.nc = nc
        ndma = ndma or {"sp": 36, "act": 12, "pool": 36}
        self.sems = []
        self.prog = {e: [] for e in ENGS}
        self.csem = {}
        self.ccnt = {e: 0 for e in ENGS}
        self.pending = {e: None for e in ENGS}
        for e in ENGS:
            self.csem[e] = len(self.sems)
            self.sems.append(stack.enter_context(nc.semaphore("c_" + e)))
        self.sem2eng = {v: k for k, v in self.csem.items()}
        self.dsem = {}
        self.drr = {}
        for q, n in ndma.items():
            self.dsem[q] = []
            self.drr[q] = 0
            for i in range(n):
                self.dsem[q].append(len(self.sems))
                self.sems.append(stack.enter_context(nc.semaphore("d_%s%d" % (q, i))))
        self.dcnt = [0] * len(self.sems)
        self.known = {e: {} for e in ENGS}
        self.nops = 0

    def _need(self, eng, deps, tok):
        if tok is None:
            return
        s, v = tok
        if eng == "pe" and s == self.csem["pe"]:
            return
        if deps.get(s, 0) < v:
            deps[s] = v

    def _collect(self, eng, reads, writes):
        deps = {}
        for b in reads:
            self._need(eng, deps, b.w)
        for b in writes:
            self._need(eng, deps, b.w)
            for s, v in b.r.items():
                self._need(eng, deps, (s, v))
        out = []
        kn = self.known[eng]
        for s, v in deps.items():
            if kn.get(s, 0) >= v:
                continue
            e2 = self.sem2eng.get(s)
            if e2 is not None and v == self.ccnt[e2] + 1:
                p = self.pending[e2]
                assert p is not None
                p["inc"] = (s, 1)
                self.ccnt[e2] += 1
                self.pending[e2] = None
            kn[s] = v
            out.append((s, v))
        return out

    def _mark(self, tok, reads, writes):
        s, v = tok
        for b in reads:
            if b.r.get(s, 0) < v:
                b.r[s] = v
        for b in writes:
            b.w = tok
            b.r = {}

    def op(self, eng, fn, reads=(), writes=(), sig=False):
        waits = self._collect(eng, reads, writes)
        rec = {"waits": waits, "fn": fn, "inc": None}
        self.prog[eng].append(rec)
        if sig:
            self.ccnt[eng] += 1
            rec["inc"] = (self.csem[eng], 1)
            self.pending[eng] = None
            tok = (self.csem[eng], self.ccnt[eng])
        else:
            self.pending[eng] = rec
            tok = (self.csem[eng], self.ccnt[eng] + 1)
        self._mark(tok, reads, writes)
        self.nops += 1
        return tok

    def dma(self, q, fn, reads=(), writes=()):
        j = self.dsem[q][self.drr[q] % len(self.dsem[q])]
        self.drr[q] += 1
        waits = self._collect(q, reads, writes)
        prev = self.dcnt[j]
        if prev > 0 and self.known[q].get(j, 0) < prev:
            self.known[q][j] = prev
            waits.append((j, prev))
        self.dcnt[j] = prev + 16
        rec = {"waits": waits, "fn": fn, "inc": (j, 16)}
        self.prog[q].append(rec)
        tok = (j, prev + 16)
        self._mark(tok, reads, writes)
        self.nops += 1
        return tok

    def barrier(self):
        for e in ENGS:
            p = self.pending[e]
            if p is not None:
                p["inc"] = (self.csem[e], 1)
                self.ccnt[e] += 1
                self.pending[e] = None
        waits = []
        kn = self.known["sp"]
        for e in ENGS:
            if e == "sp":
                continue
            s, v = self.csem[e], self.ccnt[e]
            if v > 0 and kn.get(s, 0) < v:
                kn[s] = v
                waits.append((s, v))
        for q in self.dsem:
            for j in self.dsem[q]:
                v = self.dcnt[j]
                if v > 0 and kn.get(j, 0) < v:
                    kn[j] = v
                    waits.append((j, v))
        ssp = self.csem["sp"]
        self.ccnt["sp"] += 1
        val = self.ccnt["sp"]
        sem = self.sems[ssp]
        self.prog["sp"].append({"waits": waits, "fn": (lambda e: e.sem_inc(sem, 1)), "inc": None})
        for e in ENGS:
            if e == "sp":
                continue
            self.prog[e].append({"waits": [(ssp, val)], "fn": None, "inc": None})
        for e in ENGS:
            for e2 in ENGS:
                self.known[e][self.csem[e2]] = self.ccnt[e2]
            for q in self.dsem:
                for j in self.dsem[q]:
                    self.known[e][j] = self.dcnt[j]

    def emit(self):
        nc = self.nc
        self.barrier()
        sems = self.sems
        prog = self.prog

        def run(engine, lst):
            for rec in lst:
                for s, v in rec["waits"]:
                    engine.wait_ge(sems[s], v)
                if rec["fn"] is not None:
                    ins = rec["fn"](engine)
                    if rec["inc"] is not None:
                        ins.then_inc(sems[rec["inc"][0]], rec["inc"][1])

        with nc.Block() as block:
            @block.tensor
            def _(e):
                run(e, prog["pe"])

            @block.scalar
            def _(e):
                run(e, prog["act"])

            @block.vector
            def _(e):
                run(e, prog["dve"])

            @block.gpsimd
            def _(e):
                if getattr(self, "pool_init", None):
                    self.pool_init(e)
                run(e, prog["pool"])

            @block.sync
            def _(e):
                run(e, prog["sp"])


ARENA_BYTES = 204 * 1024


class Ctx:
    pass


def _isz(dt):
    return 2 if dt == BF16 else 4


def build(kinds=KINDS, debug=False):
    nc = bass.Bass("TRN2", target_bir_lowering=False)
    c = Ctx()
    c.nc = nc
    n_a = sum(1 for k in kinds if k == 0)
    n_b = sum(1 for k in kinds if k == 1)
    n_c = sum(1 for k in kinds if k == 2)
    NL = len(kinds)

    def din(name, shape, dt=F32):
        return nc.dram_tensor(name, list(shape), dt, kind="ExternalInput").ap()

    def dscr(name, shape, dt):
        return nc.dram_tensor(name, list(shape), dt, kind="Internal").ap()

    I = {}
    I["x"] = din("x", [SEQ, D])
    if n_a:
        I["wqkv"] = din("wqkv", [n_a, D, 1536])
        I["qn"] = din("qn", [n_a, 64])
        I["kn"] = din("kn", [n_a, 64])
        I["wo"] = din("wo", [n_a, D, D])
    if n_b:
        I["g_win"] = din("g_win", [n_b, D, 4096])
        I["g_ng"] = din("g_ng", [n_b, 2048])
        I["g_nb"] = din("g_nb", [n_b, 2048])
        I["g_wsT"] = din("g_wsT", [n_b, P, 8, P])
        I["g_bs"] = din("g_bs", [n_b, P, 8])
        I["g_wout"] = din("g_wout", [n_b, 2048, D])
    if n_c:
        I["c_win"] = din("c_win", [n_c, D, 3072])
        I["c_w"] = din("c_w", [n_c, P, 8, 3])
        I["c_wout"] = din("c_wout", [n_c, D, D])
    for nm in ("ln_mix_g", "ln_mix_b", "ln_ffn_g", "ln_ffn_b"):
        I[nm] = din(nm, [NL, D])
    I["router_w"] = din("router_w", [NL, D, NE])
    I["router_b"] = din("router_b", [NL, NE])
    I["e_wg"] = din("e_wg", [NL, NE, D, D])
    I["e_wl"] = din("e_wl", [NL, NE, D, D])
    I["e_bg"] = din("e_bg", [NL, P, NE, 8])
    I["e_bl"] = din("e_bl", [NL, P, NE, 8])
    I["e_wd"] = din("e_wd", [NL, NE, D, D])
    I["e_bd"] = din("e_bd", [NL, NE, D])
    I["c_ident"] = din("c_ident", [P, P])
    I["c_tri"] = din("c_tri", [P, P])
    I["c_cos"] = din("c_cos", [SEQ, 32])
    I["c_sin"] = din("c_sin", [SEQ, 32])
    I["c_ecap"] = din("c_ecap", [NL, P, NE])
    out = nc.dram_tensor("out", [SEQ, D], F32, kind="ExternalOutput").ap()

    Xs = dscr("Xs", [SEQ, D], F32)
    XTd = dscr("XTd", [NT, P, 8 * P], BF16)
    xs = dscr("xs_rows", [NROWS, D], BF16)
    ys = dscr("ys_rows", [NROWS, D], F32)

    with ExitStack() as st:
        S = Sched(nc, st)
        c.S = S
        regh = {}

        def _pool_init(e):
            regh["r"] = e.alloc_register("bc")
            e.reg_mov(regh["r"], NROWS - 1)
        S.pool_init = _pool_init
        A = st.enter_context(nc.sbuf_tensor("arena", [P, ARENA_BYTES // 4], F32))
        PSUM = st.enter_context(nc.psum_tensor("psum", [P, 4096], F32))
        pb = [Buf("bank%d" % k) for k in range(8)]

        def bank(k, n=1):
            return PSUM[:, k * 512:(k + n) * 512]

        state = {"off": 0}

        def T(shape, dt=F32):
            n = 1
            for s_ in shape[1:]:
                n *= s_
            cols = (n * _isz(dt) + 3) // 4
            off = state["off"]
            state["off"] = off + cols
            assert state["off"] * 4 <= ARENA_BYTES, "SBUF arena overflow %d" % (state["off"] * 4)
            ap = A[0:shape[0], off:off + cols]
            if dt != F32:
                ap = ap.bitcast(dt)
                if n % 2:
                    ap = ap[:, 0:n]
            if len(shape) == 3:
                ap = ap.rearrange("p (a b) -> p a b", b=shape[2])
            elif len(shape) == 4:
                ap = ap.rearrange("p (a b c) -> p a b c", b=shape[2], c=shape[3])
            return ap

        def TB(shape, dt=F32, name=""):
            return T(shape, dt), Buf(name)

        identf, b_identf = TB([P, P])
        identb, b_identb = TB([P, P], BF16)
        trib, b_trib = TB([P, P], BF16)
        onesb, b_onesb = TB([P, P], BF16)
        onesf, b_onesf = TB([P, 64])
        eps5, b_eps5 = TB([P, 1])
        eps6, b_eps6 = TB([P, 1])
        ecap, b_ecap = TB([P, NE])
        cosT, b_cos = TB([P, NT, 32])
        sinT, b_sin = TB([P, NT, 32])
        GATE, b_gate = TB([P, NT, 4])
        DEST, b_dest = TB([P, NT * 4], I32)
        cum = [TB([P, NE]), TB([P, NE])]
        tmpc, b_tmpc = TB([P, P])
        bconst = Buf("const")

        S.dma("sp", lambda e: e.dma_start(out=identf, in_=I["c_ident"]), writes=[b_identf])
        S.dma("sp", lambda e: e.dma_start(out=tmpc, in_=I["c_tri"]), writes=[b_tmpc])
        S.op("dve", lambda e: e.tensor_copy(identb, identf), reads=[b_identf], writes=[b_identb])
        S.op("dve", lambda e: e.tensor_copy(trib, tmpc), reads=[b_tmpc], writes=[b_trib])
        S.op("pool", lambda e: e.memset(onesb, 1.0), writes=[b_onesb])
        S.op("pool", lambda e: e.memset(onesf, 1.0), writes=[b_onesf])
        S.op("pool", lambda e: e.memset(eps5, LN_EPS), writes=[b_eps5])
        S.op("pool", lambda e: e.memset(eps6, QK_EPS), writes=[b_eps6])
        S.dma("sp", lambda e: e.dma_start(out=cosT, in_=I["c_cos"].rearrange("(i p) j -> p i j", p=P)), writes=[b_cos])
        S.dma("sp", lambda e: e.dma_start(out=sinT, in_=I["c_sin"].rearrange("(i p) j -> p i j", p=P)), writes=[b_sin])
        persist_off = state["off"]
        zt, b_zt = TB([P, D])
        S.op("pool", lambda e: e.memset(zt, 0.0), writes=[b_zt])
        S.dma("sp", lambda e: e.dma_start(out=ys[NSLOT:NROWS, :], in_=zt), reads=[b_zt])
        S.barrier()

        def load_w_bf16(dst, src2d, kc, bufs):
            for k in range(kc):
                S.dma("pool", lambda e, k=k: e.dma_start(out=dst[:, k, :], in_=src2d[k * P:(k + 1) * P, :]),
                      writes=bufs)

        def transposes_to_xtd(i, src_bf, b_src, psb_k, xt_sb, b_xt):
            ptb = bank(psb_k).bitcast(BF16).rearrange("p (a b) -> p a b", b=P)
            for cc in range(8):
                S.op("pe", lambda e, cc=cc: e.transpose(ptb[:, cc, :], src_bf[:, cc * P:(cc + 1) * P], identb),
                     reads=[b_src, b_identb], writes=[pb[psb_k]])
            S.op("dve", lambda e: e.tensor_copy(xt_sb, ptb), reads=[pb[psb_k]], writes=[b_xt])
            S.dma("sp", lambda e: e.dma_start(out=XTd[i], in_=xt_sb.rearrange("p a b -> p (a b)")), reads=[b_xt])

        def alloc_finish(mode, share=None):
            f = Ctx()
            f.psb = 0
            f.xr, f.b_xr = TB([P, D])
            f.v, f.b_v = TB([P, D])
            f.w, f.b_w = TB([P, D])
            f.st, f.b_st = TB([P, 2, 6])
            f.mv, f.b_mv = TB([P, 2])
            f.rs, f.b_rs = TB([P, 1])
            if share is None:
                f.lng, f.b_lng = TB([P, D])
                f.lnb, f.b_lnb = TB([P, D])
            else:
                f.lng, f.b_lng, f.lnb, f.b_lnb = share.lng, share.b_lng, share.lnb, share.b_lnb
                f.psb = 1
            f.ybf, f.b_ybf = TB([P, D], BF16)
            if mode == "mix":
                f.yT, f.b_yT = TB([P, 8, P])
                f.wr, f.b_wr = TB([P, 8, NE])
                f.brt, f.b_brt = TB([P, NE])
                f.lg, f.b_lg = TB([P, NE])
                f.t8, f.b_t8 = TB([P, 8])
                f.maskb, f.b_maskb = TB([P, NE], BF16)
                f.nm, f.b_nm = TB([P, 1])
                f.e4, f.b_e4 = TB([P, 4])
                f.s4, f.b_s4 = TB([P, 1])
                f.posf, f.b_posf = TB([P, NE])
                f.valid, f.b_valid = TB([P, NE])
                f.d2, f.b_d2 = TB([P, NE])
                f.oh, f.b_oh = TB([P, NE])
                f.destf, f.b_destf = TB([P, 4])
            else:
                f.xt_sb, f.b_xt = TB([P, 8, P], BF16)
            return f

        def finish_setup(f, L, mode):
            gname, bname = ("ln_mix_g", "ln_mix_b") if mode == "mix" else ("ln_ffn_g", "ln_ffn_b")
            S.dma("sp", lambda e: e.dma_start(out=f.lng, in_=I[gname][L].partition_broadcast(P)), writes=[f.b_lng])
            S.dma("sp", lambda e: e.dma_start(out=f.lnb, in_=I[bname][L].partition_broadcast(P)), writes=[f.b_lnb])
            if mode == "mix":
                S.dma("sp", lambda e: e.dma_start(out=f.wr, in_=I["router_w"][L].rearrange("(c p) n -> p c n", p=P)),
                      writes=[f.b_wr])
                S.dma("sp", lambda e: e.dma_start(out=f.brt, in_=I["router_b"][L].partition_broadcast(P)),
                      writes=[f.b_brt])
                S.op("pool", lambda e: e.memset(cum[0][0], 0.0), writes=[cum[0][1]])
                f.cap = CAPS[L % len(CAPS)]
                S.dma("sp", lambda e: e.dma_start(out=ecap, in_=I["c_ecap"][L]), writes=[b_ecap])

        def finish(f, i, hsrc, hbufs, mode, Xin, Xout, make_xt=True):
            rows = slice(i * P, (i + 1) * P)
            xr, v, w = f.xr, f.v, f.w
            S.dma("sp", lambda e: e.dma_start(out=xr, in_=Xin[rows, :]), writes=[f.b_xr])
            S.op("dve", lambda e: e.scalar_tensor_tensor(v, xr, ALPHA, hsrc, ALU.mult, ALU.add),
                 reads=[f.b_xr] + hbufs, writes=[f.b_v])
            for cc in range(2):
                S.op("dve", lambda e, cc=cc: e.bn_stats(f.st[:, cc, :], v[:, cc * 512:(cc + 1) * 512]),
                     reads=[f.b_v], writes=[f.b_st])
            S.op("dve", lambda e: e.bn_aggr(f.mv, f.st.rearrange("p a b -> p (a b)")), reads=[f.b_st], writes=[f.b_mv])
            S.op("act", lambda e: e.activation(f.rs, f.mv[:, 1:2], AF.Ln, bias=eps5[:, 0:1], scale=1.0),
                 reads=[f.b_mv, b_eps5], writes=[f.b_rs])
            S.op("act", lambda e: e.activation(f.rs, f.rs, AF.Exp, scale=-0.5), reads=[f.b_rs], writes=[f.b_rs])
            S.op("dve", lambda e: e.tensor_scalar(w, v, f.mv[:, 0:1], f.rs[:, 0:1], ALU.subtract, ALU.mult),
                 reads=[f.b_v, f.b_mv, f.b_rs], writes=[f.b_w])
            S.op("dve", lambda e: e.tensor_tensor(v, w, f.lng, ALU.mult), reads=[f.b_w, f.b_lng], writes=[f.b_v])
            S.op("dve", lambda e: e.tensor_tensor(w, v, f.lnb, ALU.add), reads=[f.b_v, f.b_lnb], writes=[f.b_w])
            S.dma("sp", lambda e: e.dma_start(out=Xout[rows, :], in_=w), reads=[f.b_w])
            if mode == "ffn":
                if make_xt:
                    S.op("act", lambda e: e.copy(f.ybf, w), reads=[f.b_w], writes=[f.b_ybf])
                    transposes_to_xtd(i, f.ybf, f.b_ybf, f.psb, f.xt_sb, f.b_xt)
                return
            S.op("act", lambda e: e.copy(f.ybf, w), reads=[f.b_w], writes=[f.b_ybf])
            ptf = bank(0, 2).rearrange("p (a b) -> p a b", b=P)
            for cc in range(8):
                S.op("pe", lambda e, cc=cc: e.transpose(ptf[:, cc, :], w[:, cc * P:(cc + 1) * P], identf),
                     reads=[f.b_w, b_identf], writes=[pb[0], pb[1]])
            S.op("dve", lambda e: e.tensor_copy(f.yT, ptf), reads=[pb[0], pb[1]], writes=[f.b_yT])
            plg = bank(2)[:, 0:NE]
            for cc in range(8):
                S.op("pe", lambda e, cc=cc: e.matmul(plg, f.yT[:, cc, :], f.wr[:, cc, :], start=(cc == 0), stop=(cc == 7)),
                     reads=[f.b_yT, f.b_wr], writes=[pb[2]])
            S.op("dve", lambda e: e.tensor_tensor(f.lg, plg, f.brt, ALU.add), reads=[pb[2], f.b_brt], writes=[f.b_lg])
            S.op("dve", lambda e: e.max(f.t8, f.lg), reads=[f.b_lg], writes=[f.b_t8])
            S.op("dve", lambda e: e.tensor_single_scalar(f.maskb, f.lg, f.t8[:, 3:4], ALU.is_ge),
                 reads=[f.b_lg, f.b_t8], writes=[f.b_maskb])
            S.op("dve", lambda e: e.tensor_scalar_mul(f.nm, f.t8[:, 0:1], -1.0), reads=[f.b_t8], writes=[f.b_nm])
            S.op("act", lambda e: e.activation(f.e4, f.t8[:, 0:4], AF.Exp, bias=f.nm[:, 0:1], scale=1.0),
                 reads=[f.b_t8, f.b_nm], writes=[f.b_e4])
            S.op("dve", lambda e: e.reduce_sum(f.s4, f.e4, axis=AX.X), reads=[f.b_e4], writes=[f.b_s4])
            S.op("dve", lambda e: e.reciprocal(f.s4, f.s4), reads=[f.b_s4], writes=[f.b_s4])
            S.op("dve", lambda e: e.tensor_scalar_mul(GATE[:, i, :], f.e4, f.s4[:, 0:1]),
                 reads=[f.b_e4, f.b_s4], writes=[b_gate])
            ppos = bank(3)
            S.op("pe", lambda e: e.matmul(ppos[:, 0:NE], trib, f.maskb, start=True, stop=True),
                 reads=[b_trib, f.b_maskb], writes=[pb[3]])
            S.op("pe", lambda e: e.matmul(ppos[:, 64:64 + NE], onesb, f.maskb, start=True, stop=True),
                 reads=[b_onesb, f.b_maskb], writes=[pb[3]])
            cur, b_cur = cum[i % 2]
            nxt, b_nxt = cum[(i + 1) % 2]
            S.op("dve", lambda e: e.tensor_tensor(f.posf, ppos[:, 0:NE], cur, ALU.add), reads=[pb[3], b_cur],
                 writes=[f.b_posf])
            S.op("dve", lambda e: e.tensor_tensor(nxt, ppos[:, 64:64 + NE], cur, ALU.add), reads=[pb[3], b_cur],
                 writes=[b_nxt])
            S.op("dve", lambda e: e.tensor_single_scalar(f.valid, f.posf, float(f.cap), ALU.is_lt),
                 reads=[f.b_posf], writes=[f.b_valid])
            S.op("dve", lambda e: e.tensor_tensor(f.d2, f.posf, ecap, ALU.add), reads=[f.b_posf, b_ecap],
                 writes=[f.b_d2])
            S.op("dve", lambda e: e.tensor_tensor(f.posf, f.d2, f.valid, ALU.mult), reads=[f.b_d2, f.b_valid],
                 writes=[f.b_posf])
            S.op("dve", lambda e: e.tensor_scalar_add(f.d2, f.posf, float(TRASH)), reads=[f.b_posf], writes=[f.b_d2])
            for k in range(4):
                S.op("dve", lambda e, k=k: e.tensor_single_scalar(f.oh, f.lg, f.t8[:, k:k + 1], ALU.is_equal),
                     reads=[f.b_lg, f.b_t8], writes=[f.b_oh])
                S.op("dve", lambda e: e.tensor_tensor(f.valid, f.oh, f.d2, ALU.mult), reads=[f.b_oh, f.b_d2],
                     writes=[f.b_valid])
                S.op("dve", lambda e, k=k: e.reduce_sum(f.destf[:, k:k + 1], f.valid, axis=AX.X),
                     reads=[f.b_valid], writes=[f.b_destf])
            S.op("dve", lambda e: e.tensor_copy(DEST[:, i * 4:i * 4 + 4], f.destf), reads=[f.b_destf], writes=[b_dest])
            for k in range(4):
                S.dma("pool", lambda e, k=k: e.indirect_dma_start(
                    out=xs, out_offset=bass.IndirectOffsetOnAxis(ap=DEST[:, i * 4 + k:i * 4 + k + 1], axis=0),
                    in_=f.ybf, in_offset=None, bounds_check=regh["r"], oob_is_err=False),
                    reads=[f.b_ybf, b_dest])

        def phase_init():
            state["off"] = persist_off
            xin = [TB([P, D]) for _ in range(2)]
            xbf = [TB([P, D], BF16) for _ in range(2)]
            xt_sb = [TB([P, 8, P], BF16) for _ in range(2)]
            for i in range(NT):
                k = i % 2
                S.dma("sp", lambda e, i=i, k=k: e.dma_start(out=xin[k][0], in_=I["x"][i * P:(i + 1) * P, :]),
                      writes=[xin[k][1]])
                S.op("act", lambda e, k=k: e.copy(xbf[k][0], xin[k][0]), reads=[xin[k][1]], writes=[xbf[k][1]])
                transposes_to_xtd(i, xbf[k][0], xbf[k][1], k, xt_sb[k][0], xt_sb[k][1])
            S.barrier()

        def phase_conv(L, j, Xin):
            state["off"] = persist_off
            f = alloc_finish("mix")
            GT, b_GT = TB([P, 8, SEQ], BF16)
            wout, b_wout = TB([P, 8, D], BF16)
            win = [TB([P, 8, 3 * P], BF16) for _ in range(2)]
            cw, b_cw = TB([P, 8, 3])
            cx, b_cx = TB([P, SEQ + 2])
            bg_, b_bg = TB([P, SEQ])
            xtg = [TB([P, 8, 512], BF16) for _ in range(2)]
            ctmp = [TB([P, 512]) for _ in range(2)]
            yck = [TB([P, 512]) for _ in range(2)]
            finish_setup(f, L, "mix")
            S.dma("sp", lambda e: e.dma_start(out=cw, in_=I["c_w"][j]), writes=[b_cw])
            load_w_bf16(wout, I["c_wout"][j], 8, [b_wout])
            S.op("pool", lambda e: e.memset(cx[:, 0:1], 0.0), writes=[b_cx])
            S.op("pool", lambda e: e.memset(cx[:, SEQ + 1:SEQ + 2], 0.0), writes=[b_cx])
            gcnt = 0
            for fc in range(8):
                wk, b_wk = win[fc % 2]
                for part in range(3):
                    for k in range(8):
                        S.dma("pool", lambda e, k=k, part=part, fc=fc, wk=wk: e.dma_start(
                            out=wk[:, k, part * P:(part + 1) * P],
                            in_=I["c_win"][j][k * P:(k + 1) * P, part * D + fc * P: part * D + (fc + 1) * P]),
                            writes=[b_wk])
                for tg in range(8):
                    XT, b_XT = xtg[gcnt % 2]
                    gcnt += 1
                    for t in range(4):
                        S.dma("sp", lambda e, t=t, tg=tg, XT=XT: e.dma_start(
                            out=XT[:, :, t * P:(t + 1) * P], in_=XTd[tg * 4 + t].rearrange("p (a b) -> p a b", b=P)),
                            writes=[b_XT])
                    bks = (6, 7, 3)
                    for part in range(3):
                        for k in range(8):
                            S.op("pe", lambda e, k=k, part=part, wk=wk, XT=XT, bks=bks: e.matmul(
                                bank(bks[part]), wk[:, k, part * P:(part + 1) * P], XT[:, k, :],
                                start=(k == 0), stop=(k == 7)),
                                reads=[b_wk, b_XT], writes=[pb[bks[part]]])
                    tok = slice(tg * 512, (tg + 1) * 512)
                    ct, b_ct = ctmp[tg % 2]
                    S.op("act", lambda e, tok=tok: e.copy(bg_[:, tok], bank(6)), reads=[pb[6]], writes=[b_bg])
                    S.op("act", lambda e, ct=ct: e.copy(ct, bank(7)), reads=[pb[7]], writes=[b_ct])
                    S.op("dve", lambda e, ct=ct, tg=tg: e.tensor_tensor(cx[:, 1 + tg * 512:1 + (tg + 1) * 512], bank(3), ct, ALU.mult),
                         reads=[pb[3], b_ct], writes=[b_cx])
                for tg in range(8):
                    yc, b_yc = yck[tg % 2]
                    o = tg * 512
                    S.op("dve", lambda e, fc=fc, o=o, yc=yc: e.tensor_scalar_mul(yc, cx[:, o + 1:o + 513], cw[:, fc, 1:2]),
                         reads=[b_cx, b_cw], writes=[b_yc])
                    S.op("dve", lambda e, fc=fc, o=o, yc=yc: e.scalar_tensor_tensor(yc, cx[:, o:o + 512], cw[:, fc, 0:1], yc, ALU.mult, ALU.add),
                         reads=[b_cx, b_cw, b_yc], writes=[b_yc])
                    S.op("dve", lambda e, fc=fc, o=o, yc=yc: e.scalar_tensor_tensor(yc, cx[:, o + 2:o + 514], cw[:, fc, 2:3], yc, ALU.mult, ALU.add),
                         reads=[b_cx, b_cw, b_yc], writes=[b_yc])
                    S.op("pool", lambda e, fc=fc, o=o, yc=yc: e.tensor_tensor(GT[:, fc, o:o + 512], bg_[:, o:o + 512], yc, ALU.mult),
                         reads=[b_bg, b_yc], writes=[b_GT])
            for i in range(NT):
                for nh in range(2):
                    for k in range(8):
                        S.op("pe", lambda e, i=i, nh=nh, k=k: e.matmul(
                            bank(4 + nh), GT[:, k, i * P:(i + 1) * P], wout[:, k, nh * 512:(nh + 1) * 512],
                            start=(k == 0), stop=(k == 7)), reads=[b_GT, b_wout], writes=[pb[4 + nh]])
                finish(f, i, bank(4, 2), [pb[4], pb[5]], "mix", Xin, Xs)
            S.barrier()

        def phase_attn(L, j, Xin):
            state["off"] = persist_off
            f = alloc_finish("mix")
            KT, b_KT = TB([P, 2, SEQ], BF16)
            VE, b_VE = TB([P, NT, 4, P], BF16)
            wqkv, b_wqkv = TB([P, 8, 1536], BF16)
            wo, b_wo = TB([64, 16, D], BF16)
            QT, b_QT = TB([P, 8, 512], BF16)
            OT, b_OT = TB([64, 16, 512], BF16)
            xt_t = [TB([P, 8, P], BF16) for _ in range(2)]
            gq, b_gq = TB([P, 64])
            gk, b_gk = TB([P, 64])
            sq, b_sq = TB([P, D])
            ss, b_ss = TB([P, 16])
            qn_, b_qn = TB([P, D])
            t1, b_t1 = TB([P, 512])
            t2, b_t2 = TB([P, 512])
            qr, b_qr = TB([P, D], BF16)
            vtmp, b_vtmp = TB([P, 256])
            Pt = [TB([P, 1024], BF16) for _ in range(4)]
            bc, b_bc = TB([64, 512])
            finish_setup(f, L, "mix")
            S.dma("sp", lambda e: e.dma_start(out=gq, in_=I["qn"][j].partition_broadcast(P)), writes=[b_gq])
            S.dma("sp", lambda e: e.dma_start(out=gk, in_=I["kn"][j].partition_broadcast(P)), writes=[b_gk])
            load_w_bf16(wqkv, I["wqkv"][j], 8, [b_wqkv])
            for h in range(16):
                S.dma("pool", lambda e, h=h: e.dma_start(out=wo[:, h, :], in_=I["wo"][j][h * 64:(h + 1) * 64, :]),
                      writes=[b_wo])
            S.op("pool", lambda e: e.memset(VE.rearrange("p a b c -> p (a b c)"), 1.0), writes=[b_VE])

            def rms_rope(src_ps, src_bufs, nh, gain, b_gain, i):
                W = nh * 64
                S.op("act", lambda e: e.activation(sq[:, 0:W], src_ps, AF.Square), reads=src_bufs, writes=[b_sq])
                S.op("dve", lambda e: e.tensor_reduce(ss[:, 0:nh], sq[:, 0:W].rearrange("p (h d) -> p h d", d=64),
                                                       axis=AX.X, op=ALU.add), reads=[b_sq], writes=[b_ss])
                S.op("act", lambda e: e.activation(ss[:, 0:nh], ss[:, 0:nh], AF.Ln, bias=eps6[:, 0:1], scale=1.0 / 64),
                     reads=[b_ss, b_eps6], writes=[b_ss])
                S.op("act", lambda e: e.activation(ss[:, 0:nh], ss[:, 0:nh], AF.Exp, scale=-0.5), reads=[b_ss], writes=[b_ss])
                S.op("dve", lambda e: e.tensor_tensor(
                    qn_[:, 0:W].rearrange("p (h d) -> p h d", d=64), src_ps.rearrange("p (h d) -> p h d", d=64),
                    ss[:, 0:nh].unsqueeze(2).to_broadcast([P, nh, 64]), ALU.mult),
                    reads=src_bufs + [b_ss], writes=[b_qn])
                S.op("pool", lambda e: e.tensor_tensor(
                    qn_[:, 0:W].rearrange("p (h d) -> p h d", d=64), qn_[:, 0:W].rearrange("p (h d) -> p h d", d=64),
                    gain.unsqueeze(1).to_broadcast([P, nh, 64]), ALU.mult), reads=[b_qn, b_gain], writes=[b_qn])
                x4 = qn_[:, 0:W].rearrange("p (h j t) -> p h j t", j=32, t=2)
                o4 = qr[:, 0:W].rearrange("p (h j t) -> p h j t", j=32, t=2)
                x0, x1 = x4[:, :, :, 0], x4[:, :, :, 1]
                Cb = cosT[:, i, :].unsqueeze(1).to_broadcast([P, nh, 32])
                Sb = sinT[:, i, :].unsqueeze(1).to_broadcast([P, nh, 32])
                a1 = t1[:, 0:nh * 32].rearrange("p (h j) -> p h j", j=32)
                a2 = t2[:, 0:nh * 32].rearrange("p (h j) -> p h j", j=32)
                S.op("dve", lambda e: e.tensor_tensor(a1, x0, Cb, ALU.mult), reads=[b_qn, b_cos], writes=[b_t1])
                S.op("pool", lambda e: e.tensor_tensor(a2, x1, Sb, ALU.mult), reads=[b_qn, b_sin], writes=[b_t2])
                S.op("dve", lambda e: e.tensor_tensor(o4[:, :, :, 0], a1, a2, ALU.subtract), reads=[b_t1, b_t2],
                     writes=[b_qr])
                S.op("dve", lambda e: e.tensor_tensor(a1, x0, Sb, ALU.mult), reads=[b_qn, b_sin, b_qr], writes=[b_t1])
                S.op("pool", lambda e: e.tensor_tensor(a2, x1, Cb, ALU.mult), reads=[b_qn, b_cos, b_qr], writes=[b_t2])
                S.op("dve", lambda e: e.tensor_tensor(o4[:, :, :, 1], a1, a2, ALU.add), reads=[b_t1, b_t2],
                     writes=[b_qr])

            def load_xt(i):
                xt, b_xt = xt_t[i % 2]
                S.dma("sp", lambda e: e.dma_start(out=xt, in_=XTd[i].rearrange("p (a b) -> p a b", b=P)), writes=[b_xt])
                return xt, b_xt

            for i in range(NT):
                xt, b_xt = load_xt(i)
                for k in range(8):
                    S.op("pe", lambda e, k=k, xt=xt: e.matmul(bank(6), xt[:, k, :], wqkv[:, k, 1024:1536],
                                                               start=(k == 0), stop=(k == 7)),
                         reads=[b_xt, b_wqkv], writes=[pb[6]])
                S.op("act", lambda e: e.copy(vtmp, bank(6)[:, 256:512]), reads=[pb[6]], writes=[b_vtmp])
                S.op("pool", lambda e, i=i: e.tensor_copy(VE[:, i, :, 0:64], vtmp.rearrange("p (a b) -> p a b", b=64)),
                     reads=[b_vtmp], writes=[b_VE])
                rms_rope(bank(6)[:, 0:256], [pb[6]], 4, gk, b_gk, i)
                ptb = bank(0).bitcast(BF16)
                for kp in range(2):
                    S.op("pe", lambda e, kp=kp: e.transpose(ptb[:, kp * P:(kp + 1) * P], qr[:, kp * P:(kp + 1) * P], identb),
                         reads=[b_qr, b_identb], writes=[pb[0]])
                S.op("dve", lambda e, i=i: e.tensor_copy(KT[:, :, i * P:(i + 1) * P],
                                                          ptb[:, 0:256].rearrange("p (a b) -> p a b", b=P)),
                     reads=[pb[0]], writes=[b_KT])
            pcount = 0
            for ch in range(8):
                for t in range(4):
                    i = ch * 4 + t
                    xt, b_xt = load_xt(i)
                    for nh in range(2):
                        for k in range(8):
                            S.op("pe", lambda e, k=k, nh=nh, xt=xt: e.matmul(
                                bank(4 + nh), xt[:, k, :], wqkv[:, k, nh * 512:(nh + 1) * 512],
                                start=(k == 0), stop=(k == 7)), reads=[b_xt, b_wqkv], writes=[pb[4 + nh]])
                    rms_rope(bank(4, 2), [pb[4], pb[5]], 16, gq, b_gq, i)
                    ptb = bank(0).bitcast(BF16)
                    for pr in range(8):
                        S.op("pe", lambda e, pr=pr, ptb=ptb: e.transpose(
                            ptb[:, pr * P:(pr + 1) * P], qr[:, pr * P:(pr + 1) * P], identb),
                            reads=[b_qr, b_identb], writes=[pb[0]])
                    S.op("dve", lambda e, t=t, ptb=ptb: e.tensor_copy(
                        QT[:, :, t * P:(t + 1) * P], ptb.rearrange("p (a b) -> p a b", b=P)),
                        reads=[pb[0]], writes=[b_QT])
                items = [(pr, st_) for pr in range(8) for st_ in range(NT)]
                SBK = (6, 4, 2)
                LA = 2
                deferred = []

                def emit_norm(pr, accs):
                    for half in range(2):
                        h = HPERM[pr * 2 + half]
                        ab = accs + half
                        S.op("dve", lambda e, ab=ab: e.reciprocal(bc, bank(ab)[64:128, :]), reads=[pb[ab]], writes=[b_bc])
                        S.op("dve", lambda e, ab=ab, h=h: e.tensor_tensor(OT[:, h, :], bank(ab)[0:64, :], bc, ALU.mult),
                             reads=[pb[ab], b_bc], writes=[b_OT])

                def emit_pv(n):
                    pr, st_ = items[n]
                    pt, b_pt = Pt[n % 4]
                    accs = 0
                    for half in range(2):
                        kv = (pr // 4) * 2 + half
                        S.op("pe", lambda e, kv=kv, st_=st_, pt=pt, half=half, accs=accs: e.matmul(
                            bank(accs + half), VE[:, st_, kv, :], pt[:, half * 512:(half + 1) * 512],
                            start=(st_ == 0), stop=(st_ == NT - 1)),
                            reads=[b_VE, b_pt], writes=[pb[accs + half]])
                    if st_ == NT - 1:
                        deferred.append([1, pr, accs])

                for n in range(len(items) + LA):
                    if n < len(items):
                        pr, st_ = items[n]
                        kp = pr // 4
                        sb0 = SBK[n % 3]
                        pt, b_pt = Pt[n % 4]
                        for half in range(2):
                            rows = slice(half * 64, (half + 1) * 64)
                            S.op("pe", lambda e, kp=kp, st_=st_, pr=pr, sb0=sb0, half=half, rows=rows: e.matmul(
                                bank(sb0 + half), KT[rows, kp, st_ * P:(st_ + 1) * P], QT[rows, pr, :], start=True, stop=True),
                                reads=[b_KT, b_QT], writes=[pb[sb0 + half]], sig=(half == 1))
                        S.op("act", lambda e, sb0=sb0, pt=pt: e.activation(pt, bank(sb0, 2), AF.Exp, scale=0.125),
                             reads=[pb[sb0], pb[sb0 + 1]], writes=[b_pt], sig=True)
                    if n >= LA:
                        emit_pv(n - LA)
                    for d in list(deferred):
                        d[0] -= 1
                        if d[0] <= 0:
                            emit_norm(d[1], d[2])
                            deferred.remove(d)
                for d in deferred:
                    emit_norm(d[1], d[2])
                for t in range(4):
                    i = ch * 4 + t
                    for nh in range(2):
                        for h in range(16):
                            S.op("pe", lambda e, h=h, nh=nh, t=t: e.matmul(
                                bank(4 + nh), OT[:, h, t * P:(t + 1) * P], wo[:, h, nh * 512:(nh + 1) * 512],
                                start=(h == 0), stop=(h == 15)), reads=[b_OT, b_wo], writes=[pb[4 + nh]])
                    finish(f, i, bank(4, 2), [pb[4], pb[5]], "mix", Xin, Xs)
            S.barrier()

        def phase_gmlp(L, j, Xin):
            state["off"] = persist_off
            f = alloc_finish("mix")
            win, b_win = TB([P, 8, 4096], BF16)
            wout, b_wout = TB([P, 16, D], BF16)
            wsT, b_wsT = TB([P, 8, P], BF16)
            wsf, b_wsf = TB([P, 8, P])
            bs, b_bs = TB([P, 8])
            ng, b_ng = TB([P, 2048])
            nb_, b_nb = TB([P, 2048])
            z, b_z = TB([P, 4096])
            vn, b_vn = TB([P, 2048])
            vnb, b_vnb = TB([P, 2048], BF16)
            gtd, b_gtd = TB([P, 2048], BF16)
            gT, b_gT = TB([P, 16, P], BF16)
            st4, b_st4 = TB([P, 4, 6])
            mv2, b_mv2 = TB([P, 2])
            rs2, b_rs2 = TB([P, 1])
            xt_t = [TB([P, 8, P], BF16) for _ in range(2)]
            finish_setup(f, L, "mix")
            load_w_bf16(win, I["g_win"][j], 8, [b_win])
            load_w_bf16(wout, I["g_wout"][j], 16, [b_wout])
            S.dma("sp", lambda e: e.dma_start(out=wsf, in_=I["g_wsT"][j]), writes=[b_wsf])
            S.op("dve", lambda e: e.tensor_copy(wsT, wsf), reads=[b_wsf], writes=[b_wsT])
            S.dma("sp", lambda e: e.dma_start(out=bs, in_=I["g_bs"][j]), writes=[b_bs])
            S.dma("sp", lambda e: e.dma_start(out=ng, in_=I["g_ng"][j].partition_broadcast(P)), writes=[b_ng])
            S.dma("sp", lambda e: e.dma_start(out=nb_, in_=I["g_nb"][j].partition_broadcast(P)), writes=[b_nb])
            for i in range(NT):
                xt, b_xt = xt_t[i % 2]
                S.dma("sp", lambda e, i=i, xt=xt: e.dma_start(out=xt, in_=XTd[i].rearrange("p (a b) -> p a b", b=P)),
                      writes=[b_xt])
                for cb in range(8):
                    bk = 6 + cb % 2
                    for k in range(8):
                        S.op("pe", lambda e, k=k, cb=cb, bk=bk, xt=xt: e.matmul(
                            bank(bk), xt[:, k, :], win[:, k, cb * 512:(cb + 1) * 512], start=(k == 0), stop=(k == 7)),
                            reads=[b_xt, b_win], writes=[pb[bk]])
                    S.op("act", lambda e, cb=cb, bk=bk: e.activation(z[:, cb * 512:(cb + 1) * 512], bank(bk), AF.Gelu),
                         reads=[pb[bk]], writes=[b_z])
                u = z[:, 0:2048]
                v = z[:, 2048:4096]
                for cc in range(4):
                    S.op("dve", lambda e, cc=cc: e.bn_stats(st4[:, cc, :], v[:, cc * 512:(cc + 1) * 512]),
                         reads=[b_z], writes=[b_st4])
                S.op("dve", lambda e: e.bn_aggr(mv2, st4.rearrange("p a b -> p (a b)")), reads=[b_st4], writes=[b_mv2])
                S.op("act", lambda e: e.activation(rs2, mv2[:, 1:2], AF.Sqrt, bias=eps5[:, 0:1], scale=1.0),
                     reads=[b_mv2, b_eps5], writes=[b_rs2])
                S.op("dve", lambda e: e.reciprocal(rs2, rs2), reads=[b_rs2], writes=[b_rs2])
                S.op("dve", lambda e: e.tensor_scalar(vn, v, mv2[:, 0:1], rs2[:, 0:1], ALU.subtract, ALU.mult),
                     reads=[b_z, b_mv2, b_rs2], writes=[b_vn])
                S.op("dve", lambda e: e.tensor_tensor(vn, vn, ng, ALU.mult), reads=[b_vn, b_ng], writes=[b_vn])
                S.op("dve", lambda e: e.tensor_tensor(vnb, vn, nb_, ALU.add), reads=[b_vn, b_nb], writes=[b_vnb])
                for g in range(8):
                    bk = g // 2
                    col = (g % 2) * 256
                    S.op("pe", lambda e, g=g, bk=bk, col=col: e.matmul(
                        bank(bk)[:, col:col + 256], wsT[:, g, :], vnb[:, g * 256:(g + 1) * 256], start=True, stop=True),
                        reads=[b_wsT, b_vnb], writes=[pb[bk]])
                for g in range(8):
                    bk = g // 2
                    col = (g % 2) * 256
                    S.op("dve", lambda e, g=g, bk=bk, col=col: e.scalar_tensor_tensor(
                        gtd[:, g * 256:(g + 1) * 256], bank(bk)[:, col:col + 256], bs[:, g:g + 1],
                        u[:, g * 256:(g + 1) * 256], ALU.add, ALU.mult), reads=[pb[bk], b_bs, b_z], writes=[b_gtd])
                ptb = bank(6, 2).bitcast(BF16).rearrange("p (a b) -> p a b", b=P)
                for fc in range(16):
                    S.op("pe", lambda e, fc=fc: e.transpose(ptb[:, fc, :], gtd[:, fc * P:(fc + 1) * P], identb),
                         reads=[b_gtd, b_identb], writes=[pb[6], pb[7]])
                S.op("act", lambda e: e.copy(gT, ptb), reads=[pb[6], pb[7]], writes=[b_gT])
                for nh in range(2):
                    for fc in range(16):
                        S.op("pe", lambda e, fc=fc, nh=nh: e.matmul(
                            bank(4 + nh), gT[:, fc, :], wout[:, fc, nh * 512:(nh + 1) * 512],
                            start=(fc == 0), stop=(fc == 15)), reads=[b_gT, b_wout], writes=[pb[4 + nh]])
                finish(f, i, bank(4, 2), [pb[4], pb[5]], "mix", Xin, Xs)
            S.barrier()

        def phase_experts(L):
            state["off"] = persist_off
            wg = [TB([P, 8, D], BF16) for _ in range(2)]
            wl = [TB([P, 8, D], BF16) for _ in range(2)]
            wd = [TB([P, 8, D], BF16) for _ in range(2)]
            bgT, b_bgT = TB([P, NE, 8])
            blT, b_blT = TB([P, NE, 8])
            CAP = CAPS[L % len(CAPS)]
            NB = CAP // P
            HC = 512
            CHUNKS = [(0, 512), (512, CAP - 512)]
            bdb = [TB([P, D]) for _ in range(2)]
            Xe = [TB([P, NB, D], BF16)] * 2
            XeT, b_XeT = TB([P, 8, CAP], BF16)
            actT, b_actT = TB([P, 8, CAP], BF16)
            gt_ = [TB([P, HC]) for _ in range(2)]
            sg = [TB([P, HC]) for _ in range(2)]
            lt = [TB([P, HC]) for _ in range(2)]
            l2 = [TB([P, HC]) for _ in range(2)]
            ysb = [TB([P, D]) for _ in range(2)]
            S.dma("sp", lambda e: e.dma_start(out=bgT, in_=I["e_bg"][L]), writes=[b_bgT])
            S.dma("sp", lambda e: e.dma_start(out=blT, in_=I["e_bl"][L]), writes=[b_blT])
            S.op("dve", lambda e: e.tensor_scalar_add(blT.rearrange("p a b -> p (a b)"), blT.rearrange("p a b -> p (a b)"), 1.0),
                 reads=[b_blT], writes=[b_blT])

            NSTG = 4
            stg = [TB([P, D]) for _ in range(NSTG)]
            wbuf = [[[Buf() for _ in range(8)] for _ in range(3)] for _ in range(2)]
            wten = [[wg[0][0], wl[0][0], wd[0][0]], [wg[1][0], wl[1][0], wd[1][0]]]
            wsrc = ["e_wg", "e_wl", "e_wd"]
            from collections import deque
            castq = deque()
            chunk_no = [0]

            def issue_chunk_dma(item):
                ex_, m_, kk_, slot = item
                sg_, b_sg = stg[slot]
                S.dma("sp", lambda e: e.dma_start(out=sg_, in_=I[wsrc[m_]][L, ex_][kk_ * P:(kk_ + 1) * P, :]),
                      writes=[b_sg])

            dmaq = deque()

            def load_expert(ex):
                k = ex % 2
                S.dma("sp", lambda e: e.dma_start(out=bdb[k][0], in_=I["e_bd"][L, ex].partition_broadcast(P)), writes=[bdb[k][1]])
                S.dma("sp", lambda e: e.dma_start(
                    out=Xe[k][0], in_=xs[ex * CAP:(ex + 1) * CAP, :].rearrange("(t p) d -> p t d", p=P)),
                    writes=[Xe[k][1]])
                for m_ in range(3):
                    for kk_ in range(8):
                        slot = chunk_no[0] % NSTG
                        chunk_no[0] += 1
                        item = (ex, m_, kk_, slot)
                        castq.append(item)
                        dmaq.append(item)
                while dmaq and (len(castq) - len(dmaq)) < NSTG:
                    issue_chunk_dma(dmaq.popleft())

            def do_casts(n):
                for _ in range(n):
                    if not castq:
                        return
                    ex_, m_, kk_, slot = castq.popleft()
                    k_ = ex_ % 2
                    sg_, b_sg = stg[slot]
                    dst = wten[k_][m_][:, kk_, :]
                    eng = "pool" if (kk_ % 4 == 3) else "act"
                    if eng == "act":
                        S.op("act", lambda e, dst=dst, sg_=sg_: e.copy(dst, sg_), reads=[b_sg], writes=[wbuf[k_][m_][kk_]])
                    else:
                        S.op("pool", lambda e, dst=dst, sg_=sg_: e.tensor_copy(dst, sg_), reads=[b_sg], writes=[wbuf[k_][m_][kk_]])
                    while dmaq and (len(castq) - len(dmaq)) < NSTG:
                        issue_chunk_dma(dmaq.popleft())

            load_expert(0)
            do_casts(24)
            cnt = 0
            ycnt = 0
            for ex in range(NE):
                k = ex % 2
                for tt in range(NB):
                    bk = tt % 2
                    ptb = bank(bk).bitcast(BF16).rearrange("p (a b) -> p a b", b=P)
                    for cc in range(8):
                        S.op("pe", lambda e, cc=cc, tt=tt, k=k, ptb=ptb: e.transpose(
                            ptb[:, cc, :], Xe[k][0][:, tt, cc * P:(cc + 1) * P], identb),
                            reads=[Xe[k][1], b_identb], writes=[pb[bk]])
                    S.op("dve", lambda e, tt=tt, ptb=ptb: e.tensor_copy(XeT[:, :, tt * P:(tt + 1) * P], ptb),
                         reads=[pb[bk]], writes=[b_XeT])
                if ex + 1 < NE:
                    load_expert(ex + 1)
                for fc in range(8):
                    for th in range(2):
                        gb_k = 2 + cnt % 2
                        lb_k = 4 + cnt % 2
                        m = cnt % 2
                        cnt += 1
                        tok = slice(CHUNKS[th][0], CHUNKS[th][0] + CHUNKS[th][1])
                        HW = CHUNKS[th][1]
                        for kk in range(8):
                            S.op("pe", lambda e, kk=kk, fc=fc, tok=tok, gb_k=gb_k, k=k, HW=HW: e.matmul(
                                bank(gb_k)[:, 0:HW], wg[k][0][:, kk, fc * P:(fc + 1) * P], XeT[:, kk, tok],
                                start=(kk == 0), stop=(kk == 7)), reads=[wbuf[k][0][kk], b_XeT], writes=[pb[gb_k]], sig=(kk == 7))
                        for kk in range(8):
                            S.op("pe", lambda e, kk=kk, fc=fc, tok=tok, lb_k=lb_k, k=k, HW=HW: e.matmul(
                                bank(lb_k)[:, 0:HW], wl[k][0][:, kk, fc * P:(fc + 1) * P], XeT[:, kk, tok],
                                start=(kk == 0), stop=(kk == 7)), reads=[wbuf[k][1][kk], b_XeT], writes=[pb[lb_k]], sig=(kk == 7))
                        S.op("dve", lambda e, gb_k=gb_k, m=m, ex=ex, fc=fc, HW=HW: e.tensor_scalar(
                            gt_[m][0][:, 0:HW], bank(gb_k)[:, 0:HW], bgT[:, ex, fc:fc + 1], 7.0, ALU.add, ALU.min),
                            reads=[pb[gb_k], b_bgT], writes=[gt_[m][1]])
                        S.op("act", lambda e, m=m, HW=HW: e.activation(sg[m][0][:, 0:HW], gt_[m][0][:, 0:HW], AF.Sigmoid, scale=1.702),
                             reads=[gt_[m][1]], writes=[sg[m][1]])
                        S.op("dve", lambda e, lb_k=lb_k, m=m, ex=ex, fc=fc, HW=HW: e.tensor_scalar(
                            lt[m][0][:, 0:HW], bank(lb_k)[:, 0:HW], blT[:, ex, fc:fc + 1], 8.0, ALU.add, ALU.min),
                            reads=[pb[lb_k], b_blT], writes=[lt[m][1]])
                        S.op("dve", lambda e, m=m, HW=HW: e.tensor_tensor(l2[m][0][:, 0:HW], sg[m][0][:, 0:HW], gt_[m][0][:, 0:HW], ALU.mult),
                             reads=[sg[m][1], gt_[m][1]], writes=[l2[m][1]])
                        S.op("dve", lambda e, m=m, fc=fc, tok=tok, HW=HW: e.scalar_tensor_tensor(
                            actT[:, fc, tok], lt[m][0][:, 0:HW], -6.0, l2[m][0][:, 0:HW], ALU.max, ALU.mult),
                            reads=[lt[m][1], l2[m][1]], writes=[b_actT])
                        do_casts(1)
                for tt in range(NB):
                    yk = ycnt % 2
                    ycnt += 1
                    for nh in range(2):
                        bk = 6 + nh
                        ncol = slice(nh * 512, (nh + 1) * 512)
                        for fc in range(8):
                            S.op("pe", lambda e, bk=bk, ncol=ncol, fc=fc, tt=tt, k=k: e.matmul(
                                bank(bk), actT[:, fc, tt * P:(tt + 1) * P], wd[k][0][:, fc, ncol],
                                start=(fc == 0), stop=(fc == 7)), reads=[b_actT, wbuf[k][2][fc]], writes=[pb[bk]], sig=(fc == 7))
                        S.op("dve", lambda e, bk=bk, ncol=ncol, yk=yk, k=k: e.tensor_tensor(
                            ysb[yk][0][:, ncol], bank(bk), bdb[k][0][:, ncol], ALU.add),
                            reads=[pb[bk], bdb[k][1]], writes=[ysb[yk][1]])
                    r0 = ex * CAP + tt * P
                    S.dma("act", lambda e, r0=r0, yk=yk: e.dma_start(out=ys[r0:r0 + P, :], in_=ysb[yk][0]),
                          reads=[ysb[yk][1]])
                    do_casts(2)
            S.barrier()

        def phase_combine(L, Xout, make_xt):
            state["off"] = persist_off
            f = alloc_finish("ffn")
            f2 = alloc_finish("ffn", share=f)
            R = [[TB([P, D]) for _ in range(4)] for _ in range(2)]
            acc = [TB([P, D]) for _ in range(2)]
            finish_setup(f, L, "ffn")
            for i in range(NT):
                k2 = i % 2
                for k in range(4):
                    S.dma("pool", lambda e, i=i, k=k, k2=k2: e.indirect_dma_start(
                        out=R[k2][k][0], out_offset=None, in_=ys,
                        in_offset=bass.IndirectOffsetOnAxis(ap=DEST[:, i * 4 + k:i * 4 + k + 1], axis=0),
                        bounds_check=regh["r"], oob_is_err=False), reads=[b_dest], writes=[R[k2][k][1]])
                a, b_a = acc[k2]
                S.op("dve", lambda e, i=i, k2=k2, a=a: e.tensor_scalar_mul(a, R[k2][0][0], GATE[:, i, 0:1]),
                     reads=[R[k2][0][1], b_gate], writes=[b_a])
                for k in range(1, 4):
                    eng = "dve"
                    S.op(eng, lambda e, i=i, k=k, k2=k2, a=a: e.scalar_tensor_tensor(
                        a, R[k2][k][0], GATE[:, i, k:k + 1], a, ALU.mult, ALU.add),
                        reads=[R[k2][k][1], b_gate, b_a], writes=[b_a])
                finish(f if i % 2 == 0 else f2, i, a, [b_a], "ffn", Xs, Xout, make_xt=make_xt)
            S.barrier()

        phase_init()
        ja = jb = jc = 0
        for L, kind in enumerate(kinds):
            Xin = I["x"] if L == 0 else Xs
            if kind == 0:
                phase_attn(L, ja, Xin)
                ja += 1
            elif kind == 1:
                phase_gmlp(L, jb, Xin)
                jb += 1
            else:
                phase_conv(L, jc, Xin)
                jc += 1
            phase_experts(L)
            last = (L == NL - 1)
            phase_combine(L, out if last else Xs, make_xt=not last)
        S.emit()
        c.nops = S.nops
    return nc


def _rope_tables():
    rows = SEQ // 64
    t = np.arange(rows * 64, dtype=np.int32)
    row = (t // 64).astype(np.float32)
    col = (t % 64).astype(np.float32)
    n_pairs = 16
    inv = (np.float32(10000.0) ** (-np.arange(n_pairs, dtype=np.float32) / np.float32(n_pairs))).astype(np.float32)
    ang = np.concatenate([row[:, None] * inv, col[:, None] * inv], -1).astype(np.float32)
    return np.cos(ang).astype(np.float32), np.sin(ang).astype(np.float32)


def prepare_shared(inp, kinds=KINDS):
    f = lambda a: np.ascontiguousarray(np.asarray(a, dtype=np.float32))
    sh = {}
    if any(k == 0 for k in kinds):
        wq = np.asarray(inp["attn_w_qkv"])
        qcols = np.concatenate([np.arange(h * 64, (h + 1) * 64) for h in HPERM])
        sh["wqkv"] = f(np.concatenate([wq[..., qcols], wq[..., 1024:]], axis=-1))
        sh["qn"] = f(inp["attn_q_norm"])
        sh["kn"] = f(inp["attn_k_norm"])
        sh["wo"] = f(inp["attn_w_o"])
    if any(k == 1 for k in kinds):
        sh["g_win"] = f(inp["gmlp_w_in"])
        sh["g_ng"] = f(inp["gmlp_norm_g"])
        sh["g_nb"] = f(inp["gmlp_norm_b"])
        sh["g_wsT"] = f(np.transpose(np.asarray(inp["gmlp_w_s"]), (0, 3, 1, 2)))
        sh["g_bs"] = f(np.transpose(np.asarray(inp["gmlp_b_s"]), (0, 2, 1)))
        sh["g_wout"] = f(inp["gmlp_w_out"])
    if any(k == 2 for k in kinds):
        sh["c_win"] = f(inp["conv_w_in"])
        cw = np.asarray(inp["conv_w"])
        sh["c_w"] = f(np.transpose(cw.reshape(cw.shape[0], 3, 8, P), (0, 3, 2, 1)))
        sh["c_wout"] = f(inp["conv_w_out"])
    for nm in ("ln_mix_g", "ln_mix_b", "ln_ffn_g", "ln_ffn_b", "router_w", "router_b"):
        sh[nm] = f(inp[nm])
    wgu = np.asarray(inp["expert_w_gate_up"])
    sh["e_wg"] = f(wgu[..., 0::2])
    sh["e_wl"] = f(wgu[..., 1::2])
    bgu = np.asarray(inp["expert_b_gate_up"])
    nl = bgu.shape[0]
    sh["e_bg"] = f(np.transpose(bgu[..., 0::2].reshape(nl, NE, 8, P), (0, 3, 1, 2)))
    sh["e_bl"] = f(np.transpose(bgu[..., 1::2].reshape(nl, NE, 8, P), (0, 3, 1, 2)))
    sh["e_wd"] = f(inp["expert_w_down"])
    sh["e_bd"] = f(inp["expert_b_down"])
    sh["c_ident"] = np.eye(P, dtype=np.float32)
    sh["c_tri"] = np.triu(np.ones((P, P), dtype=np.float32), 1)
    cs, sn = _rope_tables()
    sh["c_cos"] = cs
    sh["c_sin"] = sn
    sh["c_ecap"] = np.stack([np.broadcast_to((np.arange(NE, dtype=np.float32) * CAPS[l % len(CAPS)] - TRASH)[None, :], (P, NE))
                             for l in range(len(kinds))], 0).astype(np.float32).copy()
    return sh


_NC_CACHE = {}


def kernel(**inputs):
    x = np.asarray(inputs["x"], dtype=np.float32)
    B = x.shape[0]
    sh = prepare_shared(inputs)
    if "nc" not in _NC_CACHE:
        _NC_CACHE["nc"] = build()
    nc = _NC_CACHE["nc"]
    in_maps = []
    for b in range(B):
        m = dict(sh)
        m["x"] = np.ascontiguousarray(x[b])
        in_maps.append(m)
    res = run_bass_kernel_spmd(nc, in_maps, core_ids=list(range(B)))
    return np.stack([np.asarray(r["out"]) for r in res.results], axis=0).astype(np.float32)
```

```python
from contextlib import ExitStack
import math
import numpy as np
import concourse.bass as bass
import concourse.mybir as mybir
from concourse.bass_utils import run_bass_kernel_spmd

F32 = mybir.dt.float32
BF16 = mybir.dt.bfloat16
I32 = mybir.dt.int32
ALU = mybir.AluOpType
AF = mybir.ActivationFunctionType
AX = mybir.AxisListType

ENGS = ("pe", "act", "dve", "pool", "sp")

P = 128
D = 1024
SEQ = 4096
NT = SEQ // P
DEPTH = 4
NE = 32
CAPS = (768, 896, 896, 896)
CAPMAX = max(CAPS)
NSLOT = NE * CAPMAX
TRASH = NSLOT
NROWS = NSLOT + P
ALPHA = float((2 * DEPTH) ** 0.25)
LN_EPS = 1e-5
QK_EPS = 1e-6
KINDS = (0, 1, 2, 0)
HPERM = tuple(g * 8 + t * 4 + r for g in range(2) for r in range(4) for t in range(2))


class Buf:
    __slots__ = ("name", "w", "r")

    def __init__(self, name=""):
        self.name = name
        self.w = None
        self.r = {}


class Sched:
    def __init__(self, nc, stack, ndma=None):
        self.nc = nc
        ndma = ndma or {"sp": 36, "act": 12, "pool": 36}
        self.sems = []
        self.prog = {e: [] for e in ENGS}
        self.csem = {}
        self.ccnt = {e: 0 for e in ENGS}
        self.pending = {e: None for e in ENGS}
        for e in ENGS:
            self.csem[e] = len(self.sems)
            self.sems.append(stack.enter_context(nc.semaphore("c_" + e)))
        self.sem2eng = {v: k for k, v in self.csem.items()}
        self.dsem = {}
        self.drr = {}
        for q, n in ndma.items():
            self.dsem[q] = []
            self.drr[q] = 0
            for i in range(n):
                self.dsem[q].append(len(self.sems))
                self.sems.append(stack.enter_context(nc.semaphore("d_%s%d" % (q, i))))
        self.dcnt = [0] * len(self.sems)
        self.known = {e: {} for e in ENGS}
        self.nops = 0

    def _need(self, eng, deps, tok):
        if tok is None:
            return
        s, v = tok
        if eng == "pe" and s == self.csem["pe"]:
            return
        if deps.get(s, 0) < v:
            deps[s] = v

    def _collect(self, eng, reads, writes):
        deps = {}
        for b in reads:
            self._need(eng, deps, b.w)
        for b in writes:
            self._need(eng, deps, b.w)
            for s, v in b.r.items():
                self._need(eng, deps, (s, v))
        out = []
        kn = self.known[eng]
        for s, v in deps.items():
            if kn.get(s, 0) >= v:
                continue
            e2 = self.sem2eng.get(s)
            if e2 is not None and v == self.ccnt[e2] + 1:
                p = self.pending[e2]
                assert p is not None
                p["inc"] = (s, 1)
                self.ccnt[e2] += 1
                self.pending[e2] = None
            kn[s] = v
            out.append((s, v))
        return out

    def _mark(self, tok, reads, writes):
        s, v = tok
        for b in reads:
            if b.r.get(s, 0) < v:
                b.r[s] = v
        for b in writes:
            b.w = tok
            b.r = {}

    def op(self, eng, fn, reads=(), writes=(), sig=False):
        waits = self._collect(eng, reads, writes)
        rec = {"waits": waits, "fn": fn, "inc": None}
        self.prog[eng].append(rec)
        if sig:
            self.ccnt[eng] += 1
            rec["inc"] = (self.csem[eng], 1)
            self.pending[eng] = None
            tok = (self.csem[eng], self.ccnt[eng])
        else:
            self.pending[eng] = rec
            tok = (self.csem[eng], self.ccnt[eng] + 1)
        self._mark(tok, reads, writes)
        self.nops += 1
        return tok

    def dma(self, q, fn, reads=(), writes=()):
        j = self.dsem[q][self.drr[q] % len(self.dsem[q])]
        self.drr[q] += 1
        waits = self._collect(q, reads, writes)
        prev = self.dcnt[j]
        if prev > 0 and self.known[q].get(j, 0) < prev:
            self.known[q][j] = prev
            waits.append((j, prev))
        self.dcnt[j] = prev + 16
        rec = {"waits": waits, "fn": fn, "inc": (j, 16)}
        self.prog[q].append(rec)
        tok = (j, prev + 16)
        self._mark(tok, reads, writes)
        self.nops += 1
        return tok

    def barrier(self):
        for e in ENGS:
            p = self.pending[e]
            if p is not None:
                p["inc"] = (self.csem[e], 1)
                self.ccnt[e] += 1
                self.pending[e] = None
        waits = []
        kn = self.known["sp"]
        for e in ENGS:
            if e == "sp":
                continue
            s, v = self.csem[e], self.ccnt[e]
            if v > 0 and kn.get(s, 0) < v:
                kn[s] = v
                waits.append((s, v))
        for q in self.dsem:
            for j in self.dsem[q]:
                v = self.dcnt[j]
                if v > 0 and kn.get(j, 0) < v:
                    kn[j] = v
                    waits.append((j, v))
        ssp = self.csem["sp"]
        self.ccnt["sp"] += 1
        val = self.ccnt["sp"]
        sem = self.sems[ssp]
        self.prog["sp"].append({"waits": waits, "fn": (lambda e: e.sem_inc(sem, 1)), "inc": None})
        for e in ENGS:
            if e == "sp":
                continue
            self.prog[e].append({"waits": [(ssp, val)], "fn": None, "inc": None})
        for e in ENGS:
            for e2 in ENGS:
                self.known[e][self.csem[e2]] = self.ccnt[e2]
            for q in self.dsem:
                for j in self.dsem[q]:
                    self.known[e][j] = self.dcnt[j]

    def emit(self):
        nc = self.nc
        self.barrier()
        sems = self.sems
        prog = self.prog

        def run(engine, lst):
            for rec in lst:
                for s, v in rec["waits"]:
                    engine.wait_ge(sems[s], v)
                if rec["fn"] is not None:
                    ins = rec["fn"](engine)
                    if rec["inc"] is not None:
                        ins.then_inc(sems[rec["inc"][0]], rec["inc"][1])

        with nc.Block() as block:
            @block.tensor
            def _(e):
                run(e, prog["pe"])

            @block.scalar
            def _(e):
                run(e, prog["act"])

            @block.vector
            def _(e):
                run(e, prog["dve"])

            @block.gpsimd
            def _(e):
                if getattr(self, "pool_init", None):
                    self.pool_init(e)
                run(e, prog["pool"])

            @block.sync
            def _(e):
                run(e, prog["sp"])


ARENA_BYTES = 204 * 1024


class Ctx:
    pass


def _isz(dt):
    return 2 if dt == BF16 else 4


def build(kinds=KINDS, debug=False):
    nc = bass.Bass("TRN2", target_bir_lowering=False)
    c = Ctx()
    c.nc = nc
    n_a = sum(1 for k in kinds if k == 0)
    n_b = sum(1 for k in kinds if k == 1)
    n_c = sum(1 for k in kinds if k == 2)
    NL = len(kinds)

    def din(name, shape, dt=F32):
        return nc.dram_tensor(name, list(shape), dt, kind="ExternalInput").ap()

    def dscr(name, shape, dt):
        return nc.dram_tensor(name, list(shape), dt, kind="Internal").ap()

    I = {}
    I["x"] = din("x", [SEQ, D])
    if n_a:
        I["wqkv"] = din("wqkv", [n_a, D, 1536])
        I["qn"] = din("qn", [n_a, 64])
        I["kn"] = din("kn", [n_a, 64])
        I["wo"] = din("wo", [n_a, D, D])
    if n_b:
        I["g_win"] = din("g_win", [n_b, D, 4096])
        I["g_ng"] = din("g_ng", [n_b, 2048])
        I["g_nb"] = din("g_nb", [n_b, 2048])
        I["g_wsT"] = din("g_wsT", [n_b, P, 8, P])
        I["g_bs"] = din("g_bs", [n_b, P, 8])
        I["g_wout"] = din("g_wout", [n_b, 2048, D])
    if n_c:
        I["c_win"] = din("c_win", [n_c, D, 3072])
        I["c_w"] = din("c_w", [n_c, P, 8, 3])
        I["c_wout"] = din("c_wout", [n_c, D, D])
    for nm in ("ln_mix_g", "ln_mix_b", "ln_ffn_g", "ln_ffn_b"):
        I[nm] = din(nm, [NL, D])
    I["router_w"] = din("router_w", [NL, D, NE])
    I["router_b"] = din("router_b", [NL, NE])
    I["e_wg"] = din("e_wg", [NL, NE, D, D])
    I["e_wl"] = din("e_wl", [NL, NE, D, D])
    I["e_bg"] = din("e_bg", [NL, P, NE, 8])
    I["e_bl"] = din("e_bl", [NL, P, NE, 8])
    I["e_wd"] = din("e_wd", [NL, NE, D, D])
    I["e_bd"] = din("e_bd", [NL, NE, D])
    I["c_ident"] = din("c_ident", [P, P])
    I["c_tri"] = din("c_tri", [P, P])
    I["c_cos"] = din("c_cos", [SEQ, 32])
    I["c_sin"] = din("c_sin", [SEQ, 32])
    I["c_ecap"] = din("c_ecap", [NL, P, NE])
    out = nc.dram_tensor("out", [SEQ, D], F32, kind="ExternalOutput").ap()

    Xs = dscr("Xs", [SEQ, D], F32)
    XTd = dscr("XTd", [NT, P, 8 * P], BF16)
    xs = dscr("xs_rows", [NROWS, D], BF16)
    ys = dscr("ys_rows", [NROWS, D], F32)

    with ExitStack() as st:
        S = Sched(nc, st)
        c.S = S
        regh = {}

        def _pool_init(e):
            regh["r"] = e.alloc_register("bc")
            e.reg_mov(regh["r"], NROWS - 1)
        S.pool_init = _pool_init
        A = st.enter_context(nc.sbuf_tensor("arena", [P, ARENA_BYTES // 4], F32))
        PSUM = st.enter_context(nc.psum_tensor("psum", [P, 4096], F32))
        pb = [Buf("bank%d" % k) for k in range(8)]

        def bank(k, n=1):
            return PSUM[:, k * 512:(k + n) * 512]

        state = {"off": 0}

        def T(shape, dt=F32):
            n = 1
            for s_ in shape[1:]:
                n *= s_
            cols = (n * _isz(dt) + 3) // 4
            off = state["off"]
            state["off"] = off + cols
            assert state["off"] * 4 <= ARENA_BYTES, "SBUF arena overflow %d" % (state["off"] * 4)
            ap = A[0:shape[0], off:off + cols]
            if dt != F32:
                ap = ap.bitcast(dt)
                if n % 2:
                    ap = ap[:, 0:n]
            if len(shape) == 3:
                ap = ap.rearrange("p (a b) -> p a b", b=shape[2])
            elif len(shape) == 4:
                ap = ap.rearrange("p (a b c) -> p a b c", b=shape[2], c=shape[3])
            return ap

        def TB(shape, dt=F32, name=""):
            return T(shape, dt), Buf(name)

        identf, b_identf = TB([P, P])
        identb, b_identb = TB([P, P], BF16)
        trib, b_trib = TB([P, P], BF16)
        onesb, b_onesb = TB([P, P], BF16)
        onesf, b_onesf = TB([P, 64])
        eps5, b_eps5 = TB([P, 1])
        eps6, b_eps6 = TB([P, 1])
        ecap, b_ecap = TB([P, NE])
        cosT, b_cos = TB([P, NT, 32])
        sinT, b_sin = TB([P, NT, 32])
        GATE, b_gate = TB([P, NT, 4])
        DEST, b_dest = TB([P, NT * 4], I32)
        cum = [TB([P, NE]), TB([P, NE])]
        tmpc, b_tmpc = TB([P, P])
        bconst = Buf("const")

        S.dma("sp", lambda e: e.dma_start(out=identf, in_=I["c_ident"]), writes=[b_identf])
        S.dma("sp", lambda e: e.dma_start(out=tmpc, in_=I["c_tri"]), writes=[b_tmpc])
        S.op("dve", lambda e: e.tensor_copy(identb, identf), reads=[b_identf], writes=[b_identb])
        S.op("dve", lambda e: e.tensor_copy(trib, tmpc), reads=[b_tmpc], writes=[b_trib])
        S.op("pool", lambda e: e.memset(onesb, 1.0), writes=[b_onesb])
        S.op("pool", lambda e: e.memset(onesf, 1.0), writes=[b_onesf])
        S.op("pool", lambda e: e.memset(eps5, LN_EPS), writes=[b_eps5])
        S.op("pool", lambda e: e.memset(eps6, QK_EPS), writes=[b_eps6])
        S.dma("sp", lambda e: e.dma_start(out=cosT, in_=I["c_cos"].rearrange("(i p) j -> p i j", p=P)), writes=[b_cos])
        S.dma("sp", lambda e: e.dma_start(out=sinT, in_=I["c_sin"].rearrange("(i p) j -> p i j", p=P)), writes=[b_sin])
        persist_off = state["off"]
        zt, b_zt = TB([P, D])
        S.op("pool", lambda e: e.memset(zt, 0.0), writes=[b_zt])
        S.dma("sp", lambda e: e.dma_start(out=ys[NSLOT:NROWS, :], in_=zt), reads=[b_zt])
        S.barrier()

        def load_w_bf16(dst, src2d, kc, bufs):
            for k in range(kc):
                S.dma("pool", lambda e, k=k: e.dma_start(out=dst[:, k, :], in_=src2d[k * P:(k + 1) * P, :]),
                      writes=bufs)

        def transposes_to_xtd(i, src_bf, b_src, psb_k, xt_sb, b_xt):
            ptb = bank(psb_k).bitcast(BF16).rearrange("p (a b) -> p a b", b=P)
            for cc in range(8):
                S.op("pe", lambda e, cc=cc: e.transpose(ptb[:, cc, :], src_bf[:, cc * P:(cc + 1) * P], identb),
                     reads=[b_src, b_identb], writes=[pb[psb_k]])
            S.op("dve", lambda e: e.tensor_copy(xt_sb, ptb), reads=[pb[psb_k]], writes=[b_xt])
            S.dma("sp", lambda e: e.dma_start(out=XTd[i], in_=xt_sb.rearrange("p a b -> p (a b)")), reads=[b_xt])

        def alloc_finish(mode, share=None):
            f = Ctx()
            f.psb = 0
            f.xr, f.b_xr = TB([P, D])
            f.v, f.b_v = TB([P, D])
            f.w, f.b_w = TB([P, D])
            f.st, f.b_st = TB([P, 2, 6])
            f.mv, f.b_mv = TB([P, 2])
            f.rs, f.b_rs = TB([P, 1])
            if share is None:
                f.lng, f.b_lng = TB([P, D])
                f.lnb, f.b_lnb = TB([P, D])
            else:
                f.lng, f.b_lng, f.lnb, f.b_lnb = share.lng, share.b_lng, share.lnb, share.b_lnb
                f.psb = 1
            f.ybf, f.b_ybf = TB([P, D], BF16)
            if mode == "mix":
                f.yT, f.b_yT = TB([P, 8, P])
                f.wr, f.b_wr = TB([P, 8, NE])
                f.brt, f.b_brt = TB([P, NE])
                f.lg, f.b_lg = TB([P, NE])
                f.t8, f.b_t8 = TB([P, 8])
                f.maskb, f.b_maskb = TB([P, NE], BF16)
                f.nm, f.b_nm = TB([P, 1])
                f.e4, f.b_e4 = TB([P, 4])
                f.s4, f.b_s4 = TB([P, 1])
                f.posf, f.b_posf = TB([P, NE])
                f.valid, f.b_valid = TB([P, NE])
                f.d2, f.b_d2 = TB([P, NE])
                f.oh, f.b_oh = TB([P, NE])
                f.oh4, _ = TB([P, 4, NE])
                f.destf, f.b_destf = TB([P, 4])
            else:
                f.xt_sb, f.b_xt = TB([P, 8, P], BF16)
            return f

        def finish_setup(f, L, mode):
            gname, bname = ("ln_mix_g", "ln_mix_b") if mode == "mix" else ("ln_ffn_g", "ln_ffn_b")
            S.dma("sp", lambda e: e.dma_start(out=f.lng, in_=I[gname][L].partition_broadcast(P)), writes=[f.b_lng])
            S.dma("sp", lambda e: e.dma_start(out=f.lnb, in_=I[bname][L].partition_broadcast(P)), writes=[f.b_lnb])
            if mode == "mix":
                S.dma("sp", lambda e: e.dma_start(out=f.wr, in_=I["router_w"][L].rearrange("(c p) n -> p c n", p=P)),
                      writes=[f.b_wr])
                S.dma("sp", lambda e: e.dma_start(out=f.brt, in_=I["router_b"][L].partition_broadcast(P)),
                      writes=[f.b_brt])
                S.op("pool", lambda e: e.memset(cum[0][0], 0.0), writes=[cum[0][1]])
                f.cap = CAPS[L % len(CAPS)]
                S.dma("sp", lambda e: e.dma_start(out=ecap, in_=I["c_ecap"][L]), writes=[b_ecap])

        def finish(f, i, hsrc, hbufs, mode, Xin, Xout, make_xt=True):
            rows = slice(i * P, (i + 1) * P)
            xr, v, w = f.xr, f.v, f.w
            S.dma("sp", lambda e: e.dma_start(out=xr, in_=Xin[rows, :]), writes=[f.b_xr])
            S.op("dve", lambda e: e.scalar_tensor_tensor(v, xr, ALPHA, hsrc, ALU.mult, ALU.add),
                 reads=[f.b_xr] + hbufs, writes=[f.b_v])
            for cc in range(2):
                S.op("dve", lambda e, cc=cc: e.bn_stats(f.st[:, cc, :], v[:, cc * 512:(cc + 1) * 512]),
                     reads=[f.b_v], writes=[f.b_st])
            S.op("dve", lambda e: e.bn_aggr(f.mv, f.st.rearrange("p a b -> p (a b)")), reads=[f.b_st], writes=[f.b_mv])
            S.op("act", lambda e: e.activation(f.rs, f.mv[:, 1:2], AF.Ln, bias=eps5[:, 0:1], scale=1.0),
                 reads=[f.b_mv, b_eps5], writes=[f.b_rs])
            S.op("act", lambda e: e.activation(f.rs, f.rs, AF.Exp, scale=-0.5), reads=[f.b_rs], writes=[f.b_rs])
            S.op("dve", lambda e: e.tensor_scalar(w, v, f.mv[:, 0:1], f.rs[:, 0:1], ALU.subtract, ALU.mult),
                 reads=[f.b_v, f.b_mv, f.b_rs], writes=[f.b_w])
            S.op("dve", lambda e: e.tensor_tensor(v, w, f.lng, ALU.mult), reads=[f.b_w, f.b_lng], writes=[f.b_v])
            S.op("dve", lambda e: e.tensor_tensor(w, v, f.lnb, ALU.add), reads=[f.b_v, f.b_lnb], writes=[f.b_w])
            S.dma("sp", lambda e: e.dma_start(out=Xout[rows, :], in_=w), reads=[f.b_w])
            if mode == "ffn":
                if make_xt:
                    S.op("act", lambda e: e.copy(f.ybf, w), reads=[f.b_w], writes=[f.b_ybf])
                    transposes_to_xtd(i, f.ybf, f.b_ybf, f.psb, f.xt_sb, f.b_xt)
                return
            S.op("act", lambda e: e.copy(f.ybf, w), reads=[f.b_w], writes=[f.b_ybf])
            ptf = bank(0, 2).rearrange("p (a b) -> p a b", b=P)
            for cc in range(8):
                S.op("pe", lambda e, cc=cc: e.transpose(ptf[:, cc, :], w[:, cc * P:(cc + 1) * P], identf),
                     reads=[f.b_w, b_identf], writes=[pb[0], pb[1]])
            S.op("dve", lambda e: e.tensor_copy(f.yT, ptf), reads=[pb[0], pb[1]], writes=[f.b_yT])
            plg = bank(2)[:, 0:NE]
            for cc in range(8):
                S.op("pe", lambda e, cc=cc: e.matmul(plg, f.yT[:, cc, :], f.wr[:, cc, :], start=(cc == 0), stop=(cc == 7)),
                     reads=[f.b_yT, f.b_wr], writes=[pb[2]])
            S.op("dve", lambda e: e.tensor_tensor(f.lg, plg, f.brt, ALU.add), reads=[pb[2], f.b_brt], writes=[f.b_lg])
            S.op("dve", lambda e: e.max(f.t8, f.lg), reads=[f.b_lg], writes=[f.b_t8])
            S.op("dve", lambda e: e.tensor_single_scalar(f.maskb, f.lg, f.t8[:, 3:4], ALU.is_ge),
                 reads=[f.b_lg, f.b_t8], writes=[f.b_maskb])
            S.op("dve", lambda e: e.tensor_scalar_mul(f.nm, f.t8[:, 0:1], -1.0), reads=[f.b_t8], writes=[f.b_nm])
            S.op("act", lambda e: e.activation(f.e4, f.t8[:, 0:4], AF.Exp, bias=f.nm[:, 0:1], scale=1.0),
                 reads=[f.b_t8, f.b_nm], writes=[f.b_e4])
            S.op("dve", lambda e: e.reduce_sum(f.s4, f.e4, axis=AX.X), reads=[f.b_e4], writes=[f.b_s4])
            S.op("dve", lambda e: e.reciprocal(f.s4, f.s4), reads=[f.b_s4], writes=[f.b_s4])
            S.op("dve", lambda e: e.tensor_scalar_mul(GATE[:, i, :], f.e4, f.s4[:, 0:1]),
                 reads=[f.b_e4, f.b_s4], writes=[b_gate])
            ppos = bank(3)
            S.op("pe", lambda e: e.matmul(ppos[:, 0:NE], trib, f.maskb, start=True, stop=True),
                 reads=[b_trib, f.b_maskb], writes=[pb[3]])
            S.op("pe", lambda e: e.matmul(ppos[:, 64:64 + NE], onesb, f.maskb, start=True, stop=True),
                 reads=[b_onesb, f.b_maskb], writes=[pb[3]])
            cur, b_cur = cum[i % 2]
            nxt, b_nxt = cum[(i + 1) % 2]
            S.op("dve", lambda e: e.tensor_tensor(f.posf, ppos[:, 0:NE], cur, ALU.add), reads=[pb[3], b_cur],
                 writes=[f.b_posf])
            S.op("dve", lambda e: e.tensor_tensor(nxt, ppos[:, 64:64 + NE], cur, ALU.add), reads=[pb[3], b_cur],
                 writes=[b_nxt])
            S.op("dve", lambda e: e.tensor_single_scalar(f.valid, f.posf, float(f.cap), ALU.is_lt),
                 reads=[f.b_posf], writes=[f.b_valid])
            S.op("dve", lambda e: e.tensor_tensor(f.d2, f.posf, ecap, ALU.add), reads=[f.b_posf, b_ecap],
                 writes=[f.b_d2])
            S.op("dve", lambda e: e.tensor_tensor(f.posf, f.d2, f.valid, ALU.mult), reads=[f.b_d2, f.b_valid],
                 writes=[f.b_posf])
            S.op("dve", lambda e: e.tensor_scalar_add(f.d2, f.posf, float(TRASH)), reads=[f.b_posf], writes=[f.b_d2])
            S.op("dve", lambda e: e.tensor_tensor(
                f.oh4, f.lg.unsqueeze(1).to_broadcast([P, 4, NE]), f.t8[:, 0:4].unsqueeze(2).to_broadcast([P, 4, NE]),
                ALU.is_equal), reads=[f.b_lg, f.b_t8], writes=[f.b_oh])
            S.op("dve", lambda e: e.tensor_tensor(f.oh4, f.oh4, f.d2.unsqueeze(1).to_broadcast([P, 4, NE]), ALU.mult),
                 reads=[f.b_oh, f.b_d2], writes=[f.b_oh])
            S.op("dve", lambda e: e.tensor_reduce(f.destf, f.oh4, axis=AX.X, op=ALU.add), reads=[f.b_oh], writes=[f.b_destf])
            S.op("dve", lambda e: e.tensor_copy(DEST[:, i * 4:i * 4 + 4], f.destf), reads=[f.b_destf], writes=[b_dest])
            for k in range(4):
                S.dma("pool", lambda e, k=k: e.indirect_dma_start(
                    out=xs, out_offset=bass.IndirectOffsetOnAxis(ap=DEST[:, i * 4 + k:i * 4 + k + 1], axis=0),
                    in_=f.ybf, in_offset=None, bounds_check=regh["r"], oob_is_err=False),
                    reads=[f.b_ybf, b_dest])

        def phase_init():
            state["off"] = persist_off
            xin = [TB([P, D]) for _ in range(2)]
            xbf = [TB([P, D], BF16) for _ in range(2)]
            xt_sb = [TB([P, 8, P], BF16) for _ in range(2)]
            for i in range(NT):
                k = i % 2
                S.dma("sp", lambda e, i=i, k=k: e.dma_start(out=xin[k][0], in_=I["x"][i * P:(i + 1) * P, :]),
                      writes=[xin[k][1]])
                S.op("act", lambda e, k=k: e.copy(xbf[k][0], xin[k][0]), reads=[xin[k][1]], writes=[xbf[k][1]])
                transposes_to_xtd(i, xbf[k][0], xbf[k][1], k, xt_sb[k][0], xt_sb[k][1])
            S.barrier()

        def phase_conv(L, j, Xin):
            state["off"] = persist_off
            f = alloc_finish("mix")
            GT, b_GT = TB([P, 8, SEQ], BF16)
            wout, b_wout = TB([P, 8, D], BF16)
            win = [TB([P, 8, 3 * P], BF16) for _ in range(2)]
            cw, b_cw = TB([P, 8, 3])
            cx, b_cx = TB([P, SEQ + 2])
            bg_, b_bg = TB([P, SEQ])
            xtg = [TB([P, 8, 512], BF16) for _ in range(2)]
            ctmp = [TB([P, 512]) for _ in range(2)]
            yck = [TB([P, 512]) for _ in range(2)]
            finish_setup(f, L, "mix")
            S.dma("sp", lambda e: e.dma_start(out=cw, in_=I["c_w"][j]), writes=[b_cw])
            load_w_bf16(wout, I["c_wout"][j], 8, [b_wout])
            S.op("pool", lambda e: e.memset(cx[:, 0:1], 0.0), writes=[b_cx])
            S.op("pool", lambda e: e.memset(cx[:, SEQ + 1:SEQ + 2], 0.0), writes=[b_cx])
            gcnt = 0
            for fc in range(8):
                wk, b_wk = win[fc % 2]
                for part in range(3):
                    for k in range(8):
                        S.dma("pool", lambda e, k=k, part=part, fc=fc, wk=wk: e.dma_start(
                            out=wk[:, k, part * P:(part + 1) * P],
                            in_=I["c_win"][j][k * P:(k + 1) * P, part * D + fc * P: part * D + (fc + 1) * P]),
                            writes=[b_wk])
                for tg in range(8):
                    XT, b_XT = xtg[gcnt % 2]
                    gcnt += 1
                    for t in range(4):
                        S.dma("sp", lambda e, t=t, tg=tg, XT=XT: e.dma_start(
                            out=XT[:, :, t * P:(t + 1) * P], in_=XTd[tg * 4 + t].rearrange("p (a b) -> p a b", b=P)),
                            writes=[b_XT])
                    bks = (6, 7, 3)
                    for part in range(3):
                        for k in range(8):
                            S.op("pe", lambda e, k=k, part=part, wk=wk, XT=XT, bks=bks: e.matmul(
                                bank(bks[part]), wk[:, k, part * P:(part + 1) * P], XT[:, k, :],
                                start=(k == 0), stop=(k == 7)),
                                reads=[b_wk, b_XT], writes=[pb[bks[part]]])
                    tok = slice(tg * 512, (tg + 1) * 512)
                    ct, b_ct = ctmp[tg % 2]
                    S.op("act", lambda e, tok=tok: e.copy(bg_[:, tok], bank(6)), reads=[pb[6]], writes=[b_bg])
                    S.op("act", lambda e, ct=ct: e.copy(ct, bank(7)), reads=[pb[7]], writes=[b_ct])
                    S.op("dve", lambda e, ct=ct, tg=tg: e.tensor_tensor(cx[:, 1 + tg * 512:1 + (tg + 1) * 512], bank(3), ct, ALU.mult),
                         reads=[pb[3], b_ct], writes=[b_cx])
                for tg in range(8):
                    yc, b_yc = yck[tg % 2]
                    o = tg * 512
                    S.op("dve", lambda e, fc=fc, o=o, yc=yc: e.tensor_scalar_mul(yc, cx[:, o + 1:o + 513], cw[:, fc, 1:2]),
                         reads=[b_cx, b_cw], writes=[b_yc])
                    S.op("dve", lambda e, fc=fc, o=o, yc=yc: e.scalar_tensor_tensor(yc, cx[:, o:o + 512], cw[:, fc, 0:1], yc, ALU.mult, ALU.add),
                         reads=[b_cx, b_cw, b_yc], writes=[b_yc])
                    S.op("dve", lambda e, fc=fc, o=o, yc=yc: e.scalar_tensor_tensor(yc, cx[:, o + 2:o + 514], cw[:, fc, 2:3], yc, ALU.mult, ALU.add),
                         reads=[b_cx, b_cw, b_yc], writes=[b_yc])
                    S.op("pool", lambda e, fc=fc, o=o, yc=yc: e.tensor_tensor(GT[:, fc, o:o + 512], bg_[:, o:o + 512], yc, ALU.mult),
                         reads=[b_bg, b_yc], writes=[b_GT])
            for i in range(NT):
                for nh in range(2):
                    for k in range(8):
                        S.op("pe", lambda e, i=i, nh=nh, k=k: e.matmul(
                            bank(4 + nh), GT[:, k, i * P:(i + 1) * P], wout[:, k, nh * 512:(nh + 1) * 512],
                            start=(k == 0), stop=(k == 7)), reads=[b_GT, b_wout], writes=[pb[4 + nh]])
                finish(f, i, bank(4, 2), [pb[4], pb[5]], "mix", Xin, Xs)
            S.barrier()

        def phase_attn(L, j, Xin):
            state["off"] = persist_off
            f = alloc_finish("mix")
            KT, b_KT = TB([P, 2, SEQ], BF16)
            VE, b_VE = TB([P, NT, 4, P], BF16)
            wqkv, b_wqkv = TB([P, 8, 1536], BF16)
            wo, b_wo = TB([64, 16, D], BF16)
            QT, b_QT = TB([P, 8, 512], BF16)
            OT, b_OT = TB([64, 16, 512], BF16)
            xt_t = [TB([P, 8, P], BF16) for _ in range(2)]
            gq, b_gq = TB([P, 64])
            gk, b_gk = TB([P, 64])
            sq, b_sq = TB([P, D])
            ss, b_ss = TB([P, 16])
            qn_, b_qn = TB([P, D])
            t1, b_t1 = TB([P, 512])
            t2, b_t2 = TB([P, 512])
            qr, b_qr = TB([P, D], BF16)
            vtmp, b_vtmp = TB([P, 256])
            Pt = [TB([P, 1024], BF16) for _ in range(4)]
            bc, b_bc = TB([64, 512])
            finish_setup(f, L, "mix")
            S.dma("sp", lambda e: e.dma_start(out=gq, in_=I["qn"][j].partition_broadcast(P)), writes=[b_gq])
            S.dma("sp", lambda e: e.dma_start(out=gk, in_=I["kn"][j].partition_broadcast(P)), writes=[b_gk])
            load_w_bf16(wqkv, I["wqkv"][j], 8, [b_wqkv])
            for h in range(16):
                S.dma("pool", lambda e, h=h: e.dma_start(out=wo[:, h, :], in_=I["wo"][j][h * 64:(h + 1) * 64, :]),
                      writes=[b_wo])
            S.op("pool", lambda e: e.memset(VE.rearrange("p a b c -> p (a b c)"), 1.0), writes=[b_VE])

            def rms_rope(src_ps, src_bufs, nh, gain, b_gain, i):
                W = nh * 64
                S.op("act", lambda e: e.activation(sq[:, 0:W], src_ps, AF.Square), reads=src_bufs, writes=[b_sq])
                S.op("dve", lambda e: e.tensor_reduce(ss[:, 0:nh], sq[:, 0:W].rearrange("p (h d) -> p h d", d=64),
                                                       axis=AX.X, op=ALU.add), reads=[b_sq], writes=[b_ss])
                S.op("act", lambda e: e.activation(ss[:, 0:nh], ss[:, 0:nh], AF.Ln, bias=eps6[:, 0:1], scale=1.0 / 64),
                     reads=[b_ss, b_eps6], writes=[b_ss])
                S.op("act", lambda e: e.activation(ss[:, 0:nh], ss[:, 0:nh], AF.Exp, scale=-0.5), reads=[b_ss], writes=[b_ss])
                S.op("dve", lambda e: e.tensor_tensor(
                    qn_[:, 0:W].rearrange("p (h d) -> p h d", d=64), src_ps.rearrange("p (h d) -> p h d", d=64),
                    ss[:, 0:nh].unsqueeze(2).to_broadcast([P, nh, 64]), ALU.mult),
                    reads=src_bufs + [b_ss], writes=[b_qn])
                S.op("pool", lambda e: e.tensor_tensor(
                    qn_[:, 0:W].rearrange("p (h d) -> p h d", d=64), qn_[:, 0:W].rearrange("p (h d) -> p h d", d=64),
                    gain.unsqueeze(1).to_broadcast([P, nh, 64]), ALU.mult), reads=[b_qn, b_gain], writes=[b_qn])
                x4 = qn_[:, 0:W].rearrange("p (h j t) -> p h j t", j=32, t=2)
                o4 = qr[:, 0:W].rearrange("p (h j t) -> p h j t", j=32, t=2)
                x0, x1 = x4[:, :, :, 0], x4[:, :, :, 1]
                Cb = cosT[:, i, :].unsqueeze(1).to_broadcast([P, nh, 32])
                Sb = sinT[:, i, :].unsqueeze(1).to_broadcast([P, nh, 32])
                a1 = t1[:, 0:nh * 32].rearrange("p (h j) -> p h j", j=32)
                a2 = t2[:, 0:nh * 32].rearrange("p (h j) -> p h j", j=32)
                S.op("dve", lambda e: e.tensor_tensor(a1, x0, Cb, ALU.mult), reads=[b_qn, b_cos], writes=[b_t1])
                S.op("pool", lambda e: e.tensor_tensor(a2, x1, Sb, ALU.mult), reads=[b_qn, b_sin], writes=[b_t2])
                S.op("dve", lambda e: e.tensor_tensor(o4[:, :, :, 0], a1, a2, ALU.subtract), reads=[b_t1, b_t2],
                     writes=[b_qr])
                S.op("dve", lambda e: e.tensor_tensor(a1, x0, Sb, ALU.mult), reads=[b_qn, b_sin, b_qr], writes=[b_t1])
                S.op("pool", lambda e: e.tensor_tensor(a2, x1, Cb, ALU.mult), reads=[b_qn, b_cos, b_qr], writes=[b_t2])
                S.op("dve", lambda e: e.tensor_tensor(o4[:, :, :, 1], a1, a2, ALU.add), reads=[b_t1, b_t2],
                     writes=[b_qr])

            def load_xt(i):
                xt, b_xt = xt_t[i % 2]
                S.dma("sp", lambda e: e.dma_start(out=xt, in_=XTd[i].rearrange("p (a b) -> p a b", b=P)), writes=[b_xt])
                return xt, b_xt

            for i in range(NT):
                xt, b_xt = load_xt(i)
                for k in range(8):
                    S.op("pe", lambda e, k=k, xt=xt: e.matmul(bank(6), xt[:, k, :], wqkv[:, k, 1024:1536],
                                                               start=(k == 0), stop=(k == 7)),
                         reads=[b_xt, b_wqkv], writes=[pb[6]])
                S.op("act", lambda e: e.copy(vtmp, bank(6)[:, 256:512]), reads=[pb[6]], writes=[b_vtmp])
                S.op("pool", lambda e, i=i: e.tensor_copy(VE[:, i, :, 0:64], vtmp.rearrange("p (a b) -> p a b", b=64)),
                     reads=[b_vtmp], writes=[b_VE])
                rms_rope(bank(6)[:, 0:256], [pb[6]], 4, gk, b_gk, i)
                ptb = bank(0).bitcast(BF16)
                for kp in range(2):
                    S.op("pe", lambda e, kp=kp: e.transpose(ptb[:, kp * P:(kp + 1) * P], qr[:, kp * P:(kp + 1) * P], identb),
                         reads=[b_qr, b_identb], writes=[pb[0]])
                S.op("dve", lambda e, i=i: e.tensor_copy(KT[:, :, i * P:(i + 1) * P],
                                                          ptb[:, 0:256].rearrange("p (a b) -> p a b", b=P)),
                     reads=[pb[0]], writes=[b_KT])
            pcount = 0
            for ch in range(8):
                for t in range(4):
                    i = ch * 4 + t
                    xt, b_xt = load_xt(i)
                    for nh in range(2):
                        for k in range(8):
                            S.op("pe", lambda e, k=k, nh=nh, xt=xt: e.matmul(
                                bank(4 + nh), xt[:, k, :], wqkv[:, k, nh * 512:(nh + 1) * 512],
                                start=(k == 0), stop=(k == 7)), reads=[b_xt, b_wqkv], writes=[pb[4 + nh]])
                    rms_rope(bank(4, 2), [pb[4], pb[5]], 16, gq, b_gq, i)
                    ptb = bank(0).bitcast(BF16)
                    for pr in range(8):
                        S.op("pe", lambda e, pr=pr, ptb=ptb: e.transpose(
                            ptb[:, pr * P:(pr + 1) * P], qr[:, pr * P:(pr + 1) * P], identb),
                            reads=[b_qr, b_identb], writes=[pb[0]])
                    S.op("dve", lambda e, t=t, ptb=ptb: e.tensor_copy(
                        QT[:, :, t * P:(t + 1) * P], ptb.rearrange("p (a b) -> p a b", b=P)),
                        reads=[pb[0]], writes=[b_QT])
                items = [(pr, st_) for pr in range(8) for st_ in range(NT)]
                SBK = (6, 4, 2)
                LA = 2
                deferred = []

                def emit_norm(pr, accs):
                    for half in range(2):
                        h = HPERM[pr * 2 + half]
                        ab = accs + half
                        S.op("dve", lambda e, ab=ab: e.reciprocal(bc, bank(ab)[64:128, :]), reads=[pb[ab]], writes=[b_bc])
                        S.op("dve", lambda e, ab=ab, h=h: e.tensor_tensor(OT[:, h, :], bank(ab)[0:64, :], bc, ALU.mult),
                             reads=[pb[ab], b_bc], writes=[b_OT])

                def emit_pv(n):
                    pr, st_ = items[n]
                    pt, b_pt = Pt[n % 4]
                    accs = 0
                    for half in range(2):
                        kv = (pr // 4) * 2 + half
                        S.op("pe", lambda e, kv=kv, st_=st_, pt=pt, half=half, accs=accs: e.matmul(
                            bank(accs + half), VE[:, st_, kv, :], pt[:, half * 512:(half + 1) * 512],
                            start=(st_ == 0), stop=(st_ == NT - 1)),
                            reads=[b_VE, b_pt], writes=[pb[accs + half]])
                    if st_ == NT - 1:
                        deferred.append([1, pr, accs])

                for n in range(len(items) + LA):
                    if n < len(items):
                        pr, st_ = items[n]
                        kp = pr // 4
                        sb0 = SBK[n % 3]
                        pt, b_pt = Pt[n % 4]
                        for half in range(2):
                            rows = slice(half * 64, (half + 1) * 64)
                            S.op("pe", lambda e, kp=kp, st_=st_, pr=pr, sb0=sb0, half=half, rows=rows: e.matmul(
                                bank(sb0 + half), KT[rows, kp, st_ * P:(st_ + 1) * P], QT[rows, pr, :], start=True, stop=True),
                                reads=[b_KT, b_QT], writes=[pb[sb0 + half]], sig=(half == 1))
                        S.op("act", lambda e, sb0=sb0, pt=pt: e.activation(pt, bank(sb0, 2), AF.Exp, scale=0.125),
                             reads=[pb[sb0], pb[sb0 + 1]], writes=[b_pt], sig=True)
                    if n >= LA:
                        emit_pv(n - LA)
                    for d in list(deferred):
                        d[0] -= 1
                        if d[0] <= 0:
                            emit_norm(d[1], d[2])
                            deferred.remove(d)
                for d in deferred:
                    emit_norm(d[1], d[2])
                for t in range(4):
                    i = ch * 4 + t
                    for nh in range(2):
                        for h in range(16):
                            S.op("pe", lambda e, h=h, nh=nh, t=t: e.matmul(
                                bank(4 + nh), OT[:, h, t * P:(t + 1) * P], wo[:, h, nh * 512:(nh + 1) * 512],
                                start=(h == 0), stop=(h == 15)), reads=[b_OT, b_wo], writes=[pb[4 + nh]])
                    finish(f, i, bank(4, 2), [pb[4], pb[5]], "mix", Xin, Xs)
            S.barrier()

        def phase_gmlp(L, j, Xin):
            state["off"] = persist_off
            f = alloc_finish("mix")
            win, b_win = TB([P, 8, 4096], BF16)
            wout, b_wout = TB([P, 16, D], BF16)
            wsT, b_wsT = TB([P, 8, P], BF16)
            wsf, b_wsf = TB([P, 8, P])
            bs, b_bs = TB([P, 8])
            ng, b_ng = TB([P, 2048])
            nb_, b_nb = TB([P, 2048])
            z, b_z = TB([P, 4096])
            vn, b_vn = TB([P, 2048])
            vnb, b_vnb = TB([P, 2048], BF16)
            gtd, b_gtd = TB([P, 2048], BF16)
            gT, b_gT = TB([P, 16, P], BF16)
            st4, b_st4 = TB([P, 4, 6])
            mv2, b_mv2 = TB([P, 2])
            rs2, b_rs2 = TB([P, 1])
            xt_t = [TB([P, 8, P], BF16) for _ in range(2)]
            finish_setup(f, L, "mix")
            load_w_bf16(win, I["g_win"][j], 8, [b_win])
            load_w_bf16(wout, I["g_wout"][j], 16, [b_wout])
            S.dma("sp", lambda e: e.dma_start(out=wsf, in_=I["g_wsT"][j]), writes=[b_wsf])
            S.op("dve", lambda e: e.tensor_copy(wsT, wsf), reads=[b_wsf], writes=[b_wsT])
            S.dma("sp", lambda e: e.dma_start(out=bs, in_=I["g_bs"][j]), writes=[b_bs])
            S.dma("sp", lambda e: e.dma_start(out=ng, in_=I["g_ng"][j].partition_broadcast(P)), writes=[b_ng])
            S.dma("sp", lambda e: e.dma_start(out=nb_, in_=I["g_nb"][j].partition_broadcast(P)), writes=[b_nb])
            for i in range(NT):
                xt, b_xt = xt_t[i % 2]
                S.dma("sp", lambda e, i=i, xt=xt: e.dma_start(out=xt, in_=XTd[i].rearrange("p (a b) -> p a b", b=P)),
                      writes=[b_xt])
                for cb in range(8):
                    bk = 6 + cb % 2
                    for k in range(8):
                        S.op("pe", lambda e, k=k, cb=cb, bk=bk, xt=xt: e.matmul(
                            bank(bk), xt[:, k, :], win[:, k, cb * 512:(cb + 1) * 512], start=(k == 0), stop=(k == 7)),
                            reads=[b_xt, b_win], writes=[pb[bk]])
                    S.op("act", lambda e, cb=cb, bk=bk: e.activation(z[:, cb * 512:(cb + 1) * 512], bank(bk), AF.Gelu),
                         reads=[pb[bk]], writes=[b_z])
                u = z[:, 0:2048]
                v = z[:, 2048:4096]
                for cc in range(4):
                    S.op("dve", lambda e, cc=cc: e.bn_stats(st4[:, cc, :], v[:, cc * 512:(cc + 1) * 512]),
                         reads=[b_z], writes=[b_st4])
                S.op("dve", lambda e: e.bn_aggr(mv2, st4.rearrange("p a b -> p (a b)")), reads=[b_st4], writes=[b_mv2])
                S.op("act", lambda e: e.activation(rs2, mv2[:, 1:2], AF.Sqrt, bias=eps5[:, 0:1], scale=1.0),
                     reads=[b_mv2, b_eps5], writes=[b_rs2])
                S.op("dve", lambda e: e.reciprocal(rs2, rs2), reads=[b_rs2], writes=[b_rs2])
                S.op("dve", lambda e: e.tensor_scalar(vn, v, mv2[:, 0:1], rs2[:, 0:1], ALU.subtract, ALU.mult),
                     reads=[b_z, b_mv2, b_rs2], writes=[b_vn])
                S.op("dve", lambda e: e.tensor_tensor(vn, vn, ng, ALU.mult), reads=[b_vn, b_ng], writes=[b_vn])
                S.op("dve", lambda e: e.tensor_tensor(vnb, vn, nb_, ALU.add), reads=[b_vn, b_nb], writes=[b_vnb])
                for g in range(8):
                    bk = g // 2
                    col = (g % 2) * 256
                    S.op("pe", lambda e, g=g, bk=bk, col=col: e.matmul(
                        bank(bk)[:, col:col + 256], wsT[:, g, :], vnb[:, g * 256:(g + 1) * 256], start=True, stop=True),
                        reads=[b_wsT, b_vnb], writes=[pb[bk]])
                for g in range(8):
                    bk = g // 2
                    col = (g % 2) * 256
                    S.op("dve", lambda e, g=g, bk=bk, col=col: e.scalar_tensor_tensor(
                        gtd[:, g * 256:(g + 1) * 256], bank(bk)[:, col:col + 256], bs[:, g:g + 1],
                        u[:, g * 256:(g + 1) * 256], ALU.add, ALU.mult), reads=[pb[bk], b_bs, b_z], writes=[b_gtd])
                ptb = bank(6, 2).bitcast(BF16).rearrange("p (a b) -> p a b", b=P)
                for fc in range(16):
                    S.op("pe", lambda e, fc=fc: e.transpose(ptb[:, fc, :], gtd[:, fc * P:(fc + 1) * P], identb),
                         reads=[b_gtd, b_identb], writes=[pb[6], pb[7]])
                S.op("act", lambda e: e.copy(gT, ptb), reads=[pb[6], pb[7]], writes=[b_gT])
                for nh in range(2):
                    for fc in range(16):
                        S.op("pe", lambda e, fc=fc, nh=nh: e.matmul(
                            bank(4 + nh), gT[:, fc, :], wout[:, fc, nh * 512:(nh + 1) * 512],
                            start=(fc == 0), stop=(fc == 15)), reads=[b_gT, b_wout], writes=[pb[4 + nh]])
                finish(f, i, bank(4, 2), [pb[4], pb[5]], "mix", Xin, Xs)
            S.barrier()

        def phase_experts(L):
            state["off"] = persist_off
            wg = [TB([P, 8, D], BF16) for _ in range(2)]
            wl = [TB([P, 8, D], BF16) for _ in range(2)]
            wd = [TB([P, 8, D], BF16) for _ in range(2)]
            bgT, b_bgT = TB([P, NE, 8])
            blT, b_blT = TB([P, NE, 8])
            CAP = CAPS[L % len(CAPS)]
            NB = CAP // P
            HC = 512
            CHUNKS = [(0, 512), (512, CAP - 512)]
            bdb = [TB([P, D]) for _ in range(2)]
            Xe = [TB([P, NB, D], BF16)] * 2
            XeT, b_XeT = TB([P, 8, CAP], BF16)
            actT, b_actT = TB([P, 8, CAP], BF16)
            gt_ = [TB([P, HC]) for _ in range(2)]
            sg = [TB([P, HC]) for _ in range(2)]
            lt = [TB([P, HC]) for _ in range(2)]
            l2 = [TB([P, HC]) for _ in range(2)]
            ysb = [TB([P, D]) for _ in range(2)]
            S.dma("sp", lambda e: e.dma_start(out=bgT, in_=I["e_bg"][L]), writes=[b_bgT])
            S.dma("sp", lambda e: e.dma_start(out=blT, in_=I["e_bl"][L]), writes=[b_blT])
            S.op("dve", lambda e: e.tensor_scalar_add(blT.rearrange("p a b -> p (a b)"), blT.rearrange("p a b -> p (a b)"), 1.0),
                 reads=[b_blT], writes=[b_blT])

            NSTG = 4
            stg = [TB([P, D]) for _ in range(NSTG)]
            wbuf = [[[Buf() for _ in range(8)] for _ in range(3)] for _ in range(2)]
            wten = [[wg[0][0], wl[0][0], wd[0][0]], [wg[1][0], wl[1][0], wd[1][0]]]
            wsrc = ["e_wg", "e_wl", "e_wd"]
            from collections import deque
            castq = deque()
            chunk_no = [0]

            def issue_chunk_dma(item):
                ex_, m_, kk_, slot = item
                sg_, b_sg = stg[slot]
                S.dma("sp", lambda e: e.dma_start(out=sg_, in_=I[wsrc[m_]][L, ex_][kk_ * P:(kk_ + 1) * P, :]),
                      writes=[b_sg])

            dmaq = deque()

            def load_expert(ex):
                k = ex % 2
                S.dma("sp", lambda e: e.dma_start(out=bdb[k][0], in_=I["e_bd"][L, ex].partition_broadcast(P)), writes=[bdb[k][1]])
                S.dma("sp", lambda e: e.dma_start(
                    out=Xe[k][0], in_=xs[ex * CAP:(ex + 1) * CAP, :].rearrange("(t p) d -> p t d", p=P)),
                    writes=[Xe[k][1]])
                for m_ in range(3):
                    for kk_ in range(8):
                        slot = chunk_no[0] % NSTG
                        chunk_no[0] += 1
                        item = (ex, m_, kk_, slot)
                        castq.append(item)
                        dmaq.append(item)
                while dmaq and (len(castq) - len(dmaq)) < NSTG:
                    issue_chunk_dma(dmaq.popleft())

            def do_casts(n):
                for _ in range(n):
                    if not castq:
                        return
                    ex_, m_, kk_, slot = castq.popleft()
                    k_ = ex_ % 2
                    sg_, b_sg = stg[slot]
                    dst = wten[k_][m_][:, kk_, :]
                    eng = "pool" if (kk_ % 4 == 3) else "act"
                    if eng == "act":
                        S.op("act", lambda e, dst=dst, sg_=sg_: e.copy(dst, sg_), reads=[b_sg], writes=[wbuf[k_][m_][kk_]])
                    else:
                        S.op("pool", lambda e, dst=dst, sg_=sg_: e.tensor_copy(dst, sg_), reads=[b_sg], writes=[wbuf[k_][m_][kk_]])
                    while dmaq and (len(castq) - len(dmaq)) < NSTG:
                        issue_chunk_dma(dmaq.popleft())

            load_expert(0)
            do_casts(24)
            cnt = 0
            ycnt = 0
            for ex in range(NE):
                k = ex % 2
                for tt in range(NB):
                    bk = tt % 2
                    ptb = bank(bk).bitcast(BF16).rearrange("p (a b) -> p a b", b=P)
                    for cc in range(8):
                        S.op("pe", lambda e, cc=cc, tt=tt, k=k, ptb=ptb: e.transpose(
                            ptb[:, cc, :], Xe[k][0][:, tt, cc * P:(cc + 1) * P], identb),
                            reads=[Xe[k][1], b_identb], writes=[pb[bk]])
                    S.op("dve", lambda e, tt=tt, ptb=ptb: e.tensor_copy(XeT[:, :, tt * P:(tt + 1) * P], ptb),
                         reads=[pb[bk]], writes=[b_XeT])
                if ex + 1 < NE:
                    load_expert(ex + 1)
                for fc in range(8):
                    for th in range(2):
                        gb_k = 2 + cnt % 2
                        lb_k = 4 + cnt % 2
                        m = cnt % 2
                        cnt += 1
                        tok = slice(CHUNKS[th][0], CHUNKS[th][0] + CHUNKS[th][1])
                        HW = CHUNKS[th][1]
                        for kk in range(8):
                            S.op("pe", lambda e, kk=kk, fc=fc, tok=tok, gb_k=gb_k, k=k, HW=HW: e.matmul(
                                bank(gb_k)[:, 0:HW], wg[k][0][:, kk, fc * P:(fc + 1) * P], XeT[:, kk, tok],
                                start=(kk == 0), stop=(kk == 7)), reads=[wbuf[k][0][kk], b_XeT], writes=[pb[gb_k]], sig=(kk == 7))
                        for kk in range(8):
                            S.op("pe", lambda e, kk=kk, fc=fc, tok=tok, lb_k=lb_k, k=k, HW=HW: e.matmul(
                                bank(lb_k)[:, 0:HW], wl[k][0][:, kk, fc * P:(fc + 1) * P], XeT[:, kk, tok],
                                start=(kk == 0), stop=(kk == 7)), reads=[wbuf[k][1][kk], b_XeT], writes=[pb[lb_k]], sig=(kk == 7))
                        S.op("dve", lambda e, gb_k=gb_k, m=m, ex=ex, fc=fc, HW=HW: e.tensor_scalar(
                            gt_[m][0][:, 0:HW], bank(gb_k)[:, 0:HW], bgT[:, ex, fc:fc + 1], 7.0, ALU.add, ALU.min),
                            reads=[pb[gb_k], b_bgT], writes=[gt_[m][1]])
                        S.op("act", lambda e, m=m, HW=HW: e.activation(sg[m][0][:, 0:HW], gt_[m][0][:, 0:HW], AF.Sigmoid, scale=1.702),
                             reads=[gt_[m][1]], writes=[sg[m][1]])
                        S.op("dve", lambda e, lb_k=lb_k, m=m, ex=ex, fc=fc, HW=HW: e.tensor_scalar(
                            lt[m][0][:, 0:HW], bank(lb_k)[:, 0:HW], blT[:, ex, fc:fc + 1], 8.0, ALU.add, ALU.min),
                            reads=[pb[lb_k], b_blT], writes=[lt[m][1]])
                        S.op("dve", lambda e, m=m, HW=HW: e.tensor_tensor(l2[m][0][:, 0:HW], sg[m][0][:, 0:HW], gt_[m][0][:, 0:HW], ALU.mult),
                             reads=[sg[m][1], gt_[m][1]], writes=[l2[m][1]])
                        S.op("dve", lambda e, m=m, fc=fc, tok=tok, HW=HW: e.scalar_tensor_tensor(
                            actT[:, fc, tok], lt[m][0][:, 0:HW], -6.0, l2[m][0][:, 0:HW], ALU.max, ALU.mult),
                            reads=[lt[m][1], l2[m][1]], writes=[b_actT])
                        do_casts(1)
                for tt in range(NB):
                    yk = ycnt % 2
                    ycnt += 1
                    for nh in range(2):
                        bk = 6 + nh
                        ncol = slice(nh * 512, (nh + 1) * 512)
                        for fc in range(8):
                            S.op("pe", lambda e, bk=bk, ncol=ncol, fc=fc, tt=tt, k=k: e.matmul(
                                bank(bk), actT[:, fc, tt * P:(tt + 1) * P], wd[k][0][:, fc, ncol],
                                start=(fc == 0), stop=(fc == 7)), reads=[b_actT, wbuf[k][2][fc]], writes=[pb[bk]], sig=(fc == 7))
                        S.op("dve", lambda e, bk=bk, ncol=ncol, yk=yk, k=k: e.tensor_tensor(
                            ysb[yk][0][:, ncol], bank(bk), bdb[k][0][:, ncol], ALU.add),
                            reads=[pb[bk], bdb[k][1]], writes=[ysb[yk][1]])
                    r0 = ex * CAP + tt * P
                    S.dma("act", lambda e, r0=r0, yk=yk: e.dma_start(out=ys[r0:r0 + P, :], in_=ysb[yk][0]),
                          reads=[ysb[yk][1]])
                    do_casts(2)
            S.barrier()

        def phase_combine(L, Xout, make_xt):
            state["off"] = persist_off
            f = alloc_finish("ffn")
            f2 = alloc_finish("ffn", share=f)
            R = [[TB([P, D]) for _ in range(4)] for _ in range(2)]
            acc = [TB([P, D]) for _ in range(2)]
            finish_setup(f, L, "ffn")
            for i in range(NT):
                k2 = i % 2
                for k in range(4):
                    S.dma("pool", lambda e, i=i, k=k, k2=k2: e.indirect_dma_start(
                        out=R[k2][k][0], out_offset=None, in_=ys,
                        in_offset=bass.IndirectOffsetOnAxis(ap=DEST[:, i * 4 + k:i * 4 + k + 1], axis=0),
                        bounds_check=regh["r"], oob_is_err=False), reads=[b_dest], writes=[R[k2][k][1]])
                a, b_a = acc[k2]
                S.op("dve", lambda e, i=i, k2=k2, a=a: e.tensor_scalar_mul(a, R[k2][0][0], GATE[:, i, 0:1]),
                     reads=[R[k2][0][1], b_gate], writes=[b_a])
                for k in range(1, 4):
                    eng = "dve"
                    S.op(eng, lambda e, i=i, k=k, k2=k2, a=a: e.scalar_tensor_tensor(
                        a, R[k2][k][0], GATE[:, i, k:k + 1], a, ALU.mult, ALU.add),
                        reads=[R[k2][k][1], b_gate, b_a], writes=[b_a])
                finish(f if i % 2 == 0 else f2, i, a, [b_a], "ffn", Xs, Xout, make_xt=make_xt)
            S.barrier()

        phase_init()
        ja = jb = jc = 0
        for L, kind in enumerate(kinds):
            Xin = I["x"] if L == 0 else Xs
            if kind == 0:
                phase_attn(L, ja, Xin)
                ja += 1
            elif kind == 1:
                phase_gmlp(L, jb, Xin)
                jb += 1
            else:
                phase_conv(L, jc, Xin)
                jc += 1
            phase_experts(L)
            last = (L == NL - 1)
            phase_combine(L, out if last else Xs, make_xt=not last)
        S.emit()
        c.nops = S.nops
    return nc


def _rope_tables():
    rows = SEQ // 64
    t = np.arange(rows * 64, dtype=np.int32)
    row = (t // 64).astype(np.float32)
    col = (t % 64).astype(np.float32)
    n_pairs = 16
    inv = (np.float32(10000.0) ** (-np.arange(n_pairs, dtype=np.float32) / np.float32(n_pairs))).astype(np.float32)
    ang = np.concatenate([row[:, None] * inv, col[:, None] * inv], -1).astype(np.float32)
    return np.cos(ang).astype(np.float32), np.sin(ang).astype(np.float32)


def prepare_shared(inp, kinds=KINDS):
    f = lambda a: np.ascontiguousarray(np.asarray(a, dtype=np.float32))
    sh = {}
    if any(k == 0 for k in kinds):
        wq = np.asarray(inp["attn_w_qkv"])
        qcols = np.concatenate([np.arange(h * 64, (h + 1) * 64) for h in HPERM])
        sh["wqkv"] = f(np.concatenate([wq[..., qcols], wq[..., 1024:]], axis=-1))
        sh["qn"] = f(inp["attn_q_norm"])
        sh["kn"] = f(inp["attn_k_norm"])
        sh["wo"] = f(inp["attn_w_o"])
    if any(k == 1 for k in kinds):
        sh["g_win"] = f(inp["gmlp_w_in"])
        sh["g_ng"] = f(inp["gmlp_norm_g"])
        sh["g_nb"] = f(inp["gmlp_norm_b"])
        sh["g_wsT"] = f(np.transpose(np.asarray(inp["gmlp_w_s"]), (0, 3, 1, 2)))
        sh["g_bs"] = f(np.transpose(np.asarray(inp["gmlp_b_s"]), (0, 2, 1)))
        sh["g_wout"] = f(inp["gmlp_w_out"])
    if any(k == 2 for k in kinds):
        sh["c_win"] = f(inp["conv_w_in"])
        cw = np.asarray(inp["conv_w"])
        sh["c_w"] = f(np.transpose(cw.reshape(cw.shape[0], 3, 8, P), (0, 3, 2, 1)))
        sh["c_wout"] = f(inp["conv_w_out"])
    for nm in ("ln_mix_g", "ln_mix_b", "ln_ffn_g", "ln_ffn_b", "router_w", "router_b"):
        sh[nm] = f(inp[nm])
    wgu = np.asarray(inp["expert_w_gate_up"])
    sh["e_wg"] = f(wgu[..., 0::2])
    sh["e_wl"] = f(wgu[..., 1::2])
    bgu = np.asarray(inp["expert_b_gate_up"])
    nl = bgu.shape[0]
    sh["e_bg"] = f(np.transpose(bgu[..., 0::2].reshape(nl, NE, 8, P), (0, 3, 1, 2)))
    sh["e_bl"] = f(np.transpose(bgu[..., 1::2].reshape(nl, NE, 8, P), (0, 3, 1, 2)))
    sh["e_wd"] = f(inp["expert_w_down"])
    sh["e_bd"] = f(inp["expert_b_down"])
    sh["c_ident"] = np.eye(P, dtype=np.float32)
    sh["c_tri"] = np.triu(np.ones((P, P), dtype=np.float32), 1)
    cs, sn = _rope_tables()
    sh["c_cos"] = cs
    sh["c_sin"] = sn
    sh["c_ecap"] = np.stack([np.broadcast_to((np.arange(NE, dtype=np.float32) * CAPS[l % len(CAPS)] - TRASH)[None, :], (P, NE))
                             for l in range(len(kinds))], 0).astype(np.float32).copy()
    return sh


_NC_CACHE = {}


def kernel(**inputs):
    x = np.asarray(inputs["x"], dtype=np.float32)
    B = x.shape[0]
    sh = prepare_shared(inputs)
    if "nc" not in _NC_CACHE:
        _NC_CACHE["nc"] = build()
    nc = _NC_CACHE["nc"]
    in_maps = []
    for b in range(B):
        m = dict(sh)
        m["x"] = np.ascontiguousarray(x[b])
        in_maps.append(m)
    res = run_bass_kernel_spmd(nc, in_maps, core_ids=list(range(B)))
    return np.stack([np.asarray(r["out"]) for r in res.results], axis=0).astype(np.float32)
```
